# Optimizing a Trainium2 kernel written in Bass

```python
import math
import jax
import jax.numpy as jnp
from jax import lax
import numpy as np

D_MODEL = 1024
BATCH = 4
SEQ = 4096
DEPTH = 2

GRID_W = 64
CTX_LEN = 256
Q_BLOCK = 128
NORM_EPS = 1e-6
ROPE_BASE = 10000.0

LRU_WIDTH = 512
LRU_BLOCKS = 8
LRU_BLOCK_W = LRU_WIDTH // LRU_BLOCKS
LRU_C = 8.0
CONV_W = 4
CONV_PAD_L = 2
LRU_A_MIN = 0.9
LRU_A_MAX = 0.999

MLA_HEADS = 4
MLA_Q_RANK = 384
MLA_KV_RANK = 256
MLA_NOPE = 128
MLA_ROPE = 64
MLA_V = 128
MLA_QK = MLA_NOPE + MLA_ROPE

EVEN_IN = 2 * LRU_WIDTH + MLA_Q_RANK + MLA_KV_RANK + MLA_ROPE
EVEN_MIX = LRU_WIDTH + MLA_HEADS * MLA_V

DIFF_HEADS = 8
DIFF_HEAD_DIM = 64
DIFF_V = 2 * DIFF_HEAD_DIM
ODD_IN = 3 * DIFF_HEADS * 2 * DIFF_HEAD_DIM
ODD_MIX = DIFF_HEADS * DIFF_V

N_EXPERTS = 32
TOP_K = 4
D_FF = 1024
SWIGLU_LIMIT = 7.0
SWIGLU_ALPHA = 1.702
MOE_BLOCK = 128

N_EVEN = (DEPTH + 1) // 2
N_ODD = DEPTH // 2

kernel_name = "hybrid_rglru_mla_diffattn_moe_dit"

F32 = jnp.float32


def _normal(key, shape, scale):
    return jax.random.normal(key, shape, F32) * scale


def rms_norm(x, g):
    xf = x.astype(F32)
    y = xf * lax.rsqrt(jnp.mean(xf * xf, axis=-1, keepdims=True) + NORM_EPS)
    return (y * g.astype(F32)).astype(x.dtype)


def axial_rope_tables(n_tokens, rot_dim):
    n_rows = n_tokens // GRID_W
    rows = jnp.repeat(jnp.arange(n_rows, dtype=F32), GRID_W)
    cols = jnp.tile(jnp.arange(GRID_W, dtype=F32), n_rows)
    axis_dim = rot_dim // 2
    inv_freq = ROPE_BASE ** (-jnp.arange(0, axis_dim, 2, dtype=F32) / axis_dim)
    ang_r = rows[:, None] * inv_freq
    ang_c = cols[:, None] * inv_freq
    ang = jnp.concatenate([ang_r, ang_r, ang_c, ang_c], axis=-1)
    return jnp.cos(ang), jnp.sin(ang)


def apply_axial_rope(x, cos, sin):
    r = x.shape[-1]
    quarter = r // 4
    xf = x.astype(F32)
    xs = xf.reshape(x.shape[:-1] + (2, 2, quarter))
    rot = jnp.stack([-xs[..., 1, :], xs[..., 0, :]], axis=-2).reshape(x.shape)
    bshape = (1, x.shape[1]) + (1,) * (x.ndim - 3) + (r,)
    return (xf * cos.reshape(bshape) + rot * sin.reshape(bshape)).astype(x.dtype)


def _to_q_blocks(q):
    b, h, s, d = q.shape
    return jnp.moveaxis(q.reshape(b, h, s // Q_BLOCK, Q_BLOCK, d), 2, 0)


def _from_q_blocks(o):
    nb, b, h, qb, d = o.shape
    return jnp.moveaxis(o, 0, 2).reshape(b, h, nb * qb, d)


def attend(q, k, v):
    scale = q.shape[-1] ** -0.5

    def one(qi):
        s = jnp.einsum('bhqd,bhkd->bhqk', qi, k).astype(F32) * scale
        p = jax.nn.softmax(s, axis=-1)
        return jnp.einsum('bhqk,bhkd->bhqd', p.astype(v.dtype), v)

    return _from_q_blocks(lax.map(one, _to_q_blocks(q)))


def diff_attend(q1, q2, k1, k2, v, lam):
    scale = q1.shape[-1] ** -0.5

    def one(qs):
        a, b = qs
        p1 = jax.nn.softmax(jnp.einsum('bhqd,bhkd->bhqk', a, k1).astype(F32) * scale, axis=-1)
        p2 = jax.nn.softmax(jnp.einsum('bhqd,bhkd->bhqk', b, k2).astype(F32) * scale, axis=-1)
        return jnp.einsum('bhqk,bhkd->bhqd', (p1 - lam * p2).astype(v.dtype), v)

    return _from_q_blocks(lax.map(one, (_to_q_blocks(q1), _to_q_blocks(q2))))


def heads_to_tokens(o):
    b, h, s, d = o.shape
    return o.transpose(0, 2, 1, 3).reshape(b, s, h * d)


def centred_dwconv(x, w, b):
    y = lax.conv_general_dilated(
        x, w[:, None, :], window_strides=(1,),
        padding=[(CONV_PAD_L, CONV_W - 1 - CONV_PAD_L)],
        dimension_numbers=('NWC', 'WIO', 'NWC'),
        feature_group_count=x.shape[-1])
    return y + b


def rglru_coeffs(xr, wa, ba, wx, bx, lam):
    b, l, w = xr.shape
    xb = xr.reshape(b, l, LRU_BLOCKS, LRU_BLOCK_W)
    r = jax.nn.sigmoid((jnp.einsum('blnd,nde->blne', xb, wa).reshape(b, l, w) + ba).astype(F32))
    i = jax.nn.sigmoid((jnp.einsum('blnd,nde->blne', xb, wx).reshape(b, l, w) + bx).astype(F32))
    log_a = -LRU_C * r * jax.nn.softplus(-lam.astype(F32))
    a = jnp.exp(log_a)
    u = jnp.sqrt(-jnp.expm1(2.0 * log_a)) * i * xr.astype(F32)
    return a, u


def linear_scan(a, u, h0, reverse):
    if reverse:
        a, u = jnp.flip(a, 1), jnp.flip(u, 1)

    def comb(left, right):
        return left[0] * right[0], right[0] * left[1] + right[1]

    a_cum, h = lax.associative_scan(comb, (a, u), axis=1)
    if h0 is not None:
        h = h + a_cum * h0[:, None, :]
    return jnp.flip(h, 1) if reverse else h


def _rope_tail(t, rope):
    cos, sin = rope
    return jnp.concatenate([t[..., :MLA_NOPE], apply_axial_rope(t[..., MLA_NOPE:], cos, sin)], axis=-1)


def mla_q(q_lat, q_norm_g, w_uq, qn_g, rope):
    b, l, _ = q_lat.shape
    q = (rms_norm(q_lat, q_norm_g) @ w_uq).reshape(b, l, MLA_HEADS, MLA_QK)
    q = rms_norm(q, qn_g)
    if rope is not None:
        q = _rope_tail(q, rope)
    return q.transpose(0, 2, 1, 3)


def mla_kv(kv_lat, k_rope, kv_norm_g, w_ukv, kn_g, rope):
    b, l, _ = kv_lat.shape
    kv = (rms_norm(kv_lat, kv_norm_g) @ w_ukv).reshape(b, l, MLA_HEADS, MLA_NOPE + MLA_V)
    k_nope, v = jnp.split(kv, [MLA_NOPE], axis=-1)
    k_r = jnp.broadcast_to(k_rope[:, :, None, :], (b, l, MLA_HEADS, MLA_ROPE))
    k = rms_norm(jnp.concatenate([k_nope, k_r], axis=-1), kn_g)
    if rope is not None:
        k = _rope_tail(k, rope)
    return k.transpose(0, 2, 1, 3), v.transpose(0, 2, 1, 3)


def lru_mla_mixer(hl, hc, w_in, w_out, conv_w, conv_b, wa, ba, wx, bx, lam,
                  q_norm_g, w_uq, kv_norm_g, w_ukv, qn_g, kn_g, need_ctx_out):
    splits = [LRU_WIDTH, 2 * LRU_WIDTH, 2 * LRU_WIDTH + MLA_Q_RANK,
              2 * LRU_WIDTH + MLA_Q_RANK + MLA_KV_RANK]
    gl, rl, qcl, kvcl, krl = jnp.split(hl @ w_in, splits, axis=-1)
    gc, rc, qcc, kvcc, krc = jnp.split(hc @ w_in, splits, axis=-1)

    xl = centred_dwconv(rl, conv_w, conv_b)
    xc = centred_dwconv(rc, conv_w, conv_b)
    rec_l, rec_c = [], []
    for d, rev in enumerate((False, True)):
        ac, uc = rglru_coeffs(xc, wa[d], ba[d], wx[d], bx[d], lam[d])
        hcs = linear_scan(ac, uc, None, rev)
        h0 = hcs[:, 0] if rev else hcs[:, -1]
        al, ul = rglru_coeffs(xl, wa[d], ba[d], wx[d], bx[d], lam[d])
        rec_l.append(linear_scan(al, ul, h0, rev))
        rec_c.append(hcs)
    lru_l = jax.nn.gelu(gl) * (rec_l[0] + rec_l[1]).astype(hl.dtype)

    rope = axial_rope_tables(hl.shape[1], MLA_ROPE)
    kl, vl = mla_kv(kvcl, krl, kv_norm_g, w_ukv, kn_g, rope)
    kc, vc = mla_kv(kvcc, krc, kv_norm_g, w_ukv, kn_g, None)
    ql = mla_q(qcl, q_norm_g, w_uq, qn_g, rope)
    mla_l = attend(ql, jnp.concatenate([kc, kl], axis=2), jnp.concatenate([vc, vl], axis=2))
    out_l = jnp.concatenate([lru_l, heads_to_tokens(mla_l)], axis=-1) @ w_out
    if not need_ctx_out:
        return out_l, None

    lru_c = jax.nn.gelu(gc) * (rec_c[0] + rec_c[1]).astype(hc.dtype)
    qc = mla_q(qcc, q_norm_g, w_uq, qn_g, None)
    mla_c = attend(qc, kc, vc)
    out_c = jnp.concatenate([lru_c, heads_to_tokens(mla_c)], axis=-1) @ w_out
    return out_l, out_c


def diff_mixer(hl, hc, w_in, w_out, qn_g, kn_g, lq1, lk1, lq2, lk2, subln_g, lam_init, need_ctx_out):
    def qkv(z, rope):
        b, l, _ = z.shape
        q, k, v = jnp.split(z, 3, axis=-1)
        q = rms_norm(q.reshape(b, l, DIFF_HEADS, 2, DIFF_HEAD_DIM), qn_g)
        k = rms_norm(k.reshape(b, l, DIFF_HEADS, 2, DIFF_HEAD_DIM), kn_g)
        if rope is not None:
            q = apply_axial_rope(q, *rope)
            k = apply_axial_rope(k, *rope)
        q = q.transpose(0, 2, 1, 3, 4)
        k = k.transpose(0, 2, 1, 3, 4)
        v = v.reshape(b, l, DIFF_HEADS, DIFF_V).transpose(0, 2, 1, 3)
        return q, k, v

    lam = (jnp.exp(jnp.sum(lq1.astype(F32) * lk1.astype(F32)))
           - jnp.exp(jnp.sum(lq2.astype(F32) * lk2.astype(F32))) + lam_init)

    def finish(o):
        o = rms_norm(o, subln_g) * (1.0 - lam_init)
        return heads_to_tokens(o) @ w_out

    rope = axial_rope_tables(hl.shape[1], DIFF_HEAD_DIM)
    ql, kl, vl = qkv(hl @ w_in, rope)
    qc, kc, vc = qkv(hc @ w_in, None)
    k_all = jnp.concatenate([kc, kl], axis=2)
    v_all = jnp.concatenate([vc, vl], axis=2)
    out_l = finish(diff_attend(ql[..., 0, :], ql[..., 1, :], k_all[..., 0, :], k_all[..., 1, :], v_all, lam))
    if not need_ctx_out:
        return out_l, None
    out_c = finish(diff_attend(qc[..., 0, :], qc[..., 1, :], kc[..., 0, :], kc[..., 1, :], vc, lam))
    return out_l, out_c


def moe_ffn(h, router_w, router_b, w_gu, b_gu, w_down, b_down):
    t, d = h.shape
    logits = (h @ router_w).astype(F32) + router_b.astype(F32)
    top_logit, top_e = lax.top_k(logits, TOP_K)
    top_w = jax.nn.softmax(top_logit, axis=-1)

    n_assign = t * TOP_K
    flat_e = top_e.reshape(-1)
    flat_tok = jnp.arange(n_assign, dtype=jnp.int32) // TOP_K
    flat_w = top_w.reshape(-1)
    order = jnp.argsort(flat_e)
    se = flat_e[order]
    counts = jnp.bincount(flat_e, length=N_EXPERTS)
    padded = (counts + MOE_BLOCK - 1) // MOE_BLOCK * MOE_BLOCK
    starts = jnp.cumsum(counts) - counts
    pends = jnp.cumsum(padded)
    pstarts = pends - padded
    dest = pstarts[se] + jnp.arange(n_assign, dtype=jnp.int32) - starts[se]

    n_blocks = -(-n_assign // MOE_BLOCK) + N_EXPERTS
    n_rows = n_blocks * MOE_BLOCK
    row_tok = jnp.zeros((n_rows,), jnp.int32).at[dest].set(flat_tok[order])
    row_w = jnp.zeros((n_rows,), F32).at[dest].set(flat_w[order])
    block_start = jnp.arange(n_blocks, dtype=jnp.int32) * MOE_BLOCK
    block_e = jnp.minimum(jnp.searchsorted(pends, block_start, side='right'), N_EXPERTS - 1)
    xs = h[row_tok].reshape(n_blocks, MOE_BLOCK, d)

    def expert_block(args):
        xb, e = args
        gu = xb @ w_gu[e] + b_gu[e]
        g, u = jnp.split(gu, 2, axis=-1)
        g = jnp.minimum(g, SWIGLU_LIMIT)
        u = jnp.clip(u, -SWIGLU_LIMIT, SWIGLU_LIMIT)
        act = (u + 1.0) * g * jax.nn.sigmoid(SWIGLU_ALPHA * g)
        return act @ w_down[e] + b_down[e]

    ys = lax.map(expert_block, (xs, block_e)).reshape(n_rows, d)
    out = jax.ops.segment_sum(ys.astype(F32) * row_w[:, None], row_tok, num_segments=t)
    return out.astype(h.dtype)


def setup_inputs(seed: int = 0) -> dict:
    key = jax.random.key(seed)
    ks = jax.random.split(key, 40)
    d = D_MODEL
    gain = lambda k, shape: 1.0 + _normal(k, shape, 0.02)
    a0 = jax.random.uniform(ks[16], (N_EVEN, 2, LRU_WIDTH), F32, LRU_A_MIN, LRU_A_MAX)
    s = a0 ** (1.0 / LRU_C)
    lru_lambda = jnp.log(s) - jnp.log1p(-s)
    return {
        "x": _normal(ks[0], (BATCH, SEQ, d), 1.0),
        "c": _normal(ks[1], (BATCH, d), 1.0),
        "ctx": _normal(ks[2], (BATCH, CTX_LEN, d), 1.0),
        "c_ctx": _normal(ks[3], (d,), 1.0),
        "mod_w": _normal(ks[4], (DEPTH, d, 6 * d), 0.5 * d ** -0.5),
        "mod_b": _normal(ks[5], (DEPTH, 6 * d), 0.02),
        "norm1_g": gain(ks[6], (DEPTH, d)),
        "norm2_g": gain(ks[7], (DEPTH, d)),
        "ev_w_in": _normal(ks[8], (N_EVEN, d, EVEN_IN), d ** -0.5),
        "ev_w_out": _normal(ks[9], (N_EVEN, EVEN_MIX, d), EVEN_MIX ** -0.5),
        "lru_conv_w": _normal(ks[10], (N_EVEN, CONV_W, LRU_WIDTH), CONV_W ** -0.5),
        "lru_conv_b": _normal(ks[11], (N_EVEN, LRU_WIDTH), 0.01),
        "lru_wa": _normal(ks[12], (N_EVEN, 2, LRU_BLOCKS, LRU_BLOCK_W, LRU_BLOCK_W), LRU_BLOCK_W ** -0.5),
        "lru_ba": _normal(ks[13], (N_EVEN, 2, LRU_WIDTH), 0.01),
        "lru_wx": _normal(ks[14], (N_EVEN, 2, LRU_BLOCKS, LRU_BLOCK_W, LRU_BLOCK_W), LRU_BLOCK_W ** -0.5),
        "lru_bx": _normal(ks[15], (N_EVEN, 2, LRU_WIDTH), 0.01),
        "lru_lambda": lru_lambda,
        "mla_q_norm_g": gain(ks[17], (N_EVEN, MLA_Q_RANK)),
        "mla_w_uq": _normal(ks[18], (N_EVEN, MLA_Q_RANK, MLA_HEADS * MLA_QK), MLA_Q_RANK ** -0.5),
        "mla_kv_norm_g": gain(ks[19], (N_EVEN, MLA_KV_RANK)),
        "mla_w_ukv": _normal(ks[20], (N_EVEN, MLA_KV_RANK, MLA_HEADS * (MLA_NOPE + MLA_V)), MLA_KV_RANK ** -0.5),
        "mla_qn_g": gain(ks[21], (N_EVEN, MLA_QK)),
        "mla_kn_g": gain(ks[22], (N_EVEN, MLA_QK)),
        "od_w_in": _normal(ks[23], (N_ODD, d, ODD_IN), d ** -0.5),
        "od_w_out": _normal(ks[24], (N_ODD, ODD_MIX, d), ODD_MIX ** -0.5),
        "diff_qn_g": gain(ks[25], (N_ODD, DIFF_HEAD_DIM)),
        "diff_kn_g": gain(ks[26], (N_ODD, DIFF_HEAD_DIM)),
        "diff_lq1": _normal(ks[27], (N_ODD, DIFF_HEAD_DIM), 0.1),
        "diff_lk1": _normal(ks[28], (N_ODD, DIFF_HEAD_DIM), 0.1),
        "diff_lq2": _normal(ks[29], (N_ODD, DIFF_HEAD_DIM), 0.1),
        "diff_lk2": _normal(ks[30], (N_ODD, DIFF_HEAD_DIM), 0.1),
        "diff_subln_g": gain(ks[31], (N_ODD, DIFF_V)),
        "router_w": _normal(ks[32], (DEPTH, d, N_EXPERTS), d ** -0.5),
        "router_b": _normal(ks[33], (DEPTH, N_EXPERTS), 0.01),
        "moe_w_gu": _normal(ks[34], (DEPTH, N_EXPERTS, d, 2 * D_FF), d ** -0.5),
        "moe_b_gu": _normal(ks[35], (DEPTH, N_EXPERTS, 2 * D_FF), 0.01),
        "moe_w_down": _normal(ks[36], (DEPTH, N_EXPERTS, D_FF, d), D_FF ** -0.5),
        "moe_b_down": _normal(ks[37], (DEPTH, N_EXPERTS, d), 0.01),
    }


def reference(x, c, ctx, c_ctx, mod_w, mod_b, norm1_g, norm2_g,
              ev_w_in, ev_w_out, lru_conv_w, lru_conv_b, lru_wa, lru_ba, lru_wx, lru_bx, lru_lambda,
              mla_q_norm_g, mla_w_uq, mla_kv_norm_g, mla_w_ukv, mla_qn_g, mla_kn_g,
              od_w_in, od_w_out, diff_qn_g, diff_kn_g, diff_lq1, diff_lk1, diff_lq2, diff_lk2, diff_subln_g,
              router_w, router_b, moe_w_gu, moe_b_gu, moe_w_down, moe_b_down):
    b, s, d = x.shape
    xl, xc = x, ctx
    for layer in range(DEPTH):
        last = layer == DEPTH - 1
        mod_l = jax.nn.silu(c) @ mod_w[layer] + mod_b[layer]
        mod_c = jax.nn.silu(c_ctx) @ mod_w[layer] + mod_b[layer]
        sh1, sc1, g1, sh2, sc2, g2 = jnp.split(mod_l[:, None, :], 6, axis=-1)
        csh1, csc1, cg1, csh2, csc2, cg2 = jnp.split(mod_c, 6, axis=-1)

        hl = rms_norm(xl, norm1_g[layer]) * (1.0 + sc1) + sh1
        hc = rms_norm(xc, norm1_g[layer]) * (1.0 + csc1) + csh1
        i = layer // 2
        if layer % 2 == 0:
            ml, mc = lru_mla_mixer(hl, hc, ev_w_in[i], ev_w_out[i], lru_conv_w[i], lru_conv_b[i],
                                   lru_wa[i], lru_ba[i], lru_wx[i], lru_bx[i], lru_lambda[i],
                                   mla_q_norm_g[i], mla_w_uq[i], mla_kv_norm_g[i], mla_w_ukv[i],
                                   mla_qn_g[i], mla_kn_g[i], not last)
        else:
            lam_init = 0.8 - 0.6 * math.exp(-0.3 * layer)
            ml, mc = diff_mixer(hl, hc, od_w_in[i], od_w_out[i], diff_qn_g[i], diff_kn_g[i],
                                diff_lq1[i], diff_lk1[i], diff_lq2[i], diff_lk2[i], diff_subln_g[i],
                                lam_init, not last)
        xl = xl + g1 * ml
        hl2 = rms_norm(xl, norm2_g[layer]) * (1.0 + sc2) + sh2
        moe_args = (router_w[layer], router_b[layer], moe_w_gu[layer], moe_b_gu[layer],
                    moe_w_down[layer], moe_b_down[layer])
        if last:
            fl = moe_ffn(hl2.reshape(b * s, d), *moe_args)
            xl = xl + g2 * fl.reshape(b, s, d)
        else:
            xc = xc + cg1 * mc
            hc2 = rms_norm(xc, norm2_g[layer]) * (1.0 + csc2) + csh2
            f = moe_ffn(jnp.concatenate([hl2.reshape(b * s, d), hc2.reshape(-1, d)], axis=0), *moe_args)
            xl = xl + g2 * f[:b * s].reshape(b, s, d)
            xc = xc + cg2 * f[b * s:].reshape(xc.shape)
    return xl
```

```python
import numpy as np
from contextlib import ExitStack
import concourse.bass as bass
import concourse.mybir as mybir
from concourse.bass_utils import run_bass_kernel_spmd

F32 = mybir.dt.float32
BF16 = mybir.dt.bfloat16
AF = mybir.ActivationFunctionType
ALU = mybir.AluOpType
AX = mybir.AxisListType

EPS = 1e-6
D = 1024
DC = 8
LRU_W = 512
QR, KVR, ROPE, NOPE, VD, MH = 384, 256, 64, 128, 128, 4
EVEN_IN = 1728
DH, DHD = 8, 64
FF = 1024
TOPK = 4
LIMIT = 7.0
ALPHA = 1.702


class Prog:
    ENGS = ('pe', 'dve', 'act', 'pool', 'sp')

    def __init__(self, nc, ndma=16):
        self.nc = nc
        self.h = {'pe': nc.tensor, 'dve': nc.vector, 'act': nc.scalar, 'pool': nc.gpsimd, 'sp': nc.sync}
        self.sem = {e: nc.alloc_semaphore(name=f"s_{e}") for e in self.ENGS}
        self.dsem = [nc.alloc_semaphore(name=f"s_d{i}") for i in range(ndma)]
        self.cnt = {e: 0 for e in self.ENGS}
        self.dcnt = [0] * ndma
        self.dnext = 0
        self.known = {e: {} for e in self.ENGS}
        self.lastw = {}
        self.readers = {}
        self.ops = {e: [] for e in self.ENGS}
        self.stack = [ExitStack()]
        self.n_inst = 0
        self.csem = []

    def sb(self, name, shape, dtype):
        self.n_alloc = getattr(self, 'n_alloc', 0) + 1
        return self.stack[-1].enter_context(self.nc.sbuf_tensor(f"sb{self.n_alloc}_{name}", list(shape), dtype))

    def ps(self, name, shape, dtype):
        return self.stack[-1].enter_context(self.nc.psum_tensor(f"ps_{name}", list(shape), dtype))

    def push(self):
        self.stack.append(ExitStack())

    def pop(self):
        self.barrier()
        self.stack.pop().close()

    def _deps(self, eng, reads, writes):
        deps = {}
        def add(t):
            if t is None:
                return
            k, v = t
            if deps.get(k, 0) < v:
                deps[k] = v
        for k in reads:
            add(self.lastw.get(k))
        for k in writes:
            add(self.lastw.get(k))
            for t in self.readers.get(k, ()):
                add(t)
        waits = []
        for k, v in deps.items():
            if k == 'pe' and eng == 'pe':
                continue
            if self.known[eng].get(k, 0) >= v:
                continue
            self.known[eng][k] = v
            waits.append((k, v))
        return waits

    def _commit(self, tok, reads, writes):
        for k in writes:
            self.lastw[k] = tok
            self.readers[k] = []
        for k in reads:
            self.readers.setdefault(k, []).append(tok)

    def op(self, eng, fn, r=(), w=()):
        waits = self._deps(eng, r, w)
        self.cnt[eng] += 1
        tok = (eng, self.cnt[eng])
        self.ops[eng].append((waits, fn, ('c', eng)))
        self._commit(tok, r, w)
        self.n_inst += 1
        return tok

    def dma(self, out, in_, r=(), w=(), q='sp', **kw):
        s = self.dnext
        self.dnext = (self.dnext + 1) % len(self.dsem)
        waits = self._deps(q, r, w)
        key = ('d', s)
        prev = 16 * self.dcnt[s]
        if prev > 0 and self.known[q].get(key, 0) < prev:
            self.known[q][key] = prev
            waits.append((key, prev))
        self.dcnt[s] += 1
        tok = (key, 16 * self.dcnt[s])
        self.ops[q].append((waits, lambda e: e.dma_start(out=out, in_=in_, **kw), ('d', s)))
        self._commit(tok, r, w)
        self.n_inst += 1
        return tok

    def coll_async(self, fn, deps=()):
        h = self.nc.alloc_semaphore(name=f"s_c{len(self.csem)}")
        self.csem.append(h)
        idx = len(self.csem) - 1
        waits = []
        for (k, v) in deps:
            if self.known['pool'].get(k, 0) < v:
                self.known['pool'][k] = v
                waits.append((k, v))
        self.ops['pool'].append((waits, fn, ('x', idx)))
        self.n_inst += 1
        return (('x', idx), 16)

    def wait_tok(self, eng, tok):
        k, v = tok
        if self.known[eng].get(k, 0) < v:
            self.known[eng][k] = v
            self.ops[eng].append(([(k, v)], None, None))

    def coll(self, fn):
        self.barrier()
        s = self.dnext
        self.dnext = (self.dnext + 1) % len(self.dsem)
        self.dcnt[s] += 1
        self.ops['pool'].append(([], fn, ('d', s)))
        self.n_inst += 1
        self.barrier()

    def barrier(self):
        for e in self.ENGS:
            waits = []
            for e2 in self.ENGS:
                if e2 != e and self.cnt[e2] > self.known[e].get(e2, 0):
                    waits.append((e2, self.cnt[e2]))
                    self.known[e][e2] = self.cnt[e2]
            for s in range(len(self.dsem)):
                v = 16 * self.dcnt[s]
                if v > self.known[e].get(('d', s), 0):
                    waits.append((('d', s), v))
                    self.known[e][('d', s)] = v
            if waits:
                self.ops[e].append((waits, None, None))
        self.lastw = {}
        self.readers = {}

    def _semh(self, k):
        if isinstance(k, str):
            return self.sem[k]
        return self.dsem[k[1]] if k[0] == 'd' else self.csem[k[1]]

    def finish(self):
        for i in range(len(self.csem)):
            self.wait_tok('sp', (('x', i), 16))
        self.barrier()
        with self.nc.Block() as block:
            def replay(ename, e):
                for waits, fn, inc in self.ops[ename]:
                    for k, v in waits:
                        e.wait_ge(self._semh(k), v)
                    if fn is None:
                        continue
                    ins = fn(e)
                    if inc[0] == 'c':
                        ins.then_inc(self.sem[inc[1]], 1)
                    elif inc[0] == 'd':
                        ins.then_inc(self.dsem[inc[1]], 16)
                    else:
                        ins.then_inc(self.csem[inc[1]], 16)

            @block.tensor
            def _(e):
                replay('pe', e)

            @block.vector
            def _(e):
                replay('dve', e)

            @block.scalar
            def _(e):
                replay('act', e)

            @block.gpsimd
            def _(e):
                replay('pool', e)

            @block.sync
            def _(e):
                replay('sp', e)
        while self.stack:
            self.stack.pop().close()

    def mm(self, out, lhsT, rhs, start=True, stop=True, r=(), w=()):
        return self.op('pe', lambda e: e.matmul(out, lhsT, rhs, start=start, stop=stop), r, w)

    def tr(self, out, in_, ident, r=(), w=()):
        return self.op('pe', lambda e: e.transpose(out, in_, ident), r, w)

    def act(self, out, in_, func, bias=None, scale=None, accum=None, r=(), w=()):
        kw = {}
        if bias is not None:
            kw['bias'] = bias
        if scale is not None:
            kw['scale'] = scale
        if accum is not None:
            kw['accum_out'] = accum
        return self.op('act', lambda e: e.activation(out=out, in_=in_, func=func, **kw), r, w)

    def tt(self, out, in0, in1, op, r=(), w=(), eng='dve'):
        return self.op(eng, lambda e: e.tensor_tensor(out=out, in0=in0, in1=in1, op=op), r, w)

    def ts(self, out, in0, s1, op0, s2=None, op1=None, r=(), w=(), eng='dve'):
        if op1 is None:
            return self.op(eng, lambda e: e.tensor_scalar(out=out, in0=in0, scalar1=s1, scalar2=None, op0=op0), r, w)
        return self.op(eng, lambda e: e.tensor_scalar(out=out, in0=in0, scalar1=s1, scalar2=s2, op0=op0, op1=op1), r, w)

    def stt(self, out, in0, scalar, in1, op0, op1, r=(), w=(), eng='dve'):
        return self.op(eng, lambda e: e.scalar_tensor_tensor(out=out, in0=in0, scalar=scalar, in1=in1, op0=op0, op1=op1), r, w)

    def recip(self, out, in_, r=(), w=()):
        return self.op('dve', lambda e: e.reciprocal(out=out, in_=in_), r, w)

    def copy(self, out, in_, r=(), w=(), eng='dve'):
        if eng == 'act':
            return self.op('act', lambda e: e.copy(out=out, in_=in_), r, w)
        return self.op(eng, lambda e: e.tensor_copy(out=out, in_=in_), r, w)

    def memset(self, ap, val, w=(), eng='dve'):
        return self.op(eng, lambda e: e.memset(ap, val), (), w)

    def scan(self, out, d0, d1, initial, r=(), w=()):
        return self.op('dve', lambda e: e.tensor_tensor_scan(out=out, data0=d0, data1=d1, initial=initial,
                                                              op0=ALU.mult, op1=ALU.add), r, w)

    def rsqrt(self, out, in_, scale, r=(), w=()):
        np_ = out.partition_size()
        self.act(out, in_, AF.Sqrt, bias=self.epsc[0:np_, 0:1], scale=scale, r=list(r) + ['epsc'], w=w)
        return self.recip(out, out, r=w, w=w)


def groups(s, e, gw=512):
    return [(t, min(gw, e - t)) for t in range(s, e, gw)]


class Ring:
    def __init__(self, P, name, shape, dtype, n=2):
        self.t = [P.sb(f"{name}{i}", shape, dtype) for i in range(n)]
        self.k = [f"{name}{i}" for i in range(n)]
        self.i = -1

    def next(self):
        self.i = (self.i + 1) % len(self.t)
        return self.t[self.i], self.k[self.i]


class LayerBuilder:
    def __init__(self, cfg, layer, shared=None):
        self.cfg = cfg
        self.layer = layer
        S, C, NE = cfg['S'], cfg['C'], cfg['NE']
        self.S, self.C, self.NE = S, C, NE
        self.NT = C + S
        self.OWN = S // 2
        self.even = (layer % 2 == 0)
        self.last = (layer == cfg['DEPTH'] - 1)
        self.Q0 = C if self.last else 0
        self.Q1 = (C + self.OWN) if self.last else self.NT
        self.TQ = self.Q1 - self.Q0
        gw = min(512, self.OWN)
        self.gA = groups(0, C, gw) + groups(C, C + self.OWN, gw) + groups(C + self.OWN, self.NT, gw)
        self.gQ = [g for g in self.gA if self.Q0 <= g[0] < self.Q1]
        self.prefix = f"L{layer}_"
        if shared is None:
            self.nc = bass.Bass("TRN2", target_bir_lowering=False)
            self.P = Prog(self.nc)
            self.io = {}
        else:
            self.nc, self.P = shared.nc, shared.P
            self.io = dict(shared.gio)
            for k in ('ident', 'identb', 'ones32', 'onesb', 'bd64', 'prot', 'pb', 'staged'):
                setattr(self, k, getattr(shared, k))
        self.gio = {}

    def din(self, name, shape, dt=F32, glob=False):
        nm = name if glob else self.prefix + name
        self.io[name] = self.nc.dram_tensor(nm, list(shape), dt, kind="ExternalInput").ap()
        if glob:
            self.gio[name] = self.io[name]
        return self.io[name]

    def dout(self, name, shape, dt=F32):
        self.io[name] = self.nc.dram_tensor(name, list(shape), dt, kind="ExternalOutput").ap()
        return self.io[name]

    def dscr(self, name, shape, dt=F32, glob=False):
        nm = name if glob else self.prefix + name
        self.io[name] = self.nc.dram_tensor(nm, list(shape), dt, kind="Internal").ap()
        if glob:
            self.gio[name] = self.io[name]
        return self.io[name]

    def setup_consts(self):
        P, io = self.P, self.io
        self.ident = P.sb("ident", [128, 128], F32)
        self.identb = P.sb("identb", [128, 128], BF16)
        self.ones32 = P.sb("ones32", [128, 128], F32)
        self.onesb = P.sb("onesb", [128, 128], BF16)
        self.bd64 = P.sb("bd64", [128, 128], BF16)
        self.protf = P.sb("protf", [128, 128], F32)
        self.prot = P.sb("prot", [128, 128], BF16)
        P.epsc = P.sb("epsc", [128, 1], F32)
        self.pb = [P.ps(f"pb{i}", [128, 512], F32) for i in range(8)]
        P.dma(self.ident[:], io['ident'][:, :], w=['ident'])
        P.dma(self.protf[:], io['prot'][:, :], w=['protf'])
        P.copy(self.identb[:], self.ident[:], r=['ident'], w=['identb'])
        P.copy(self.prot[:], self.protf[:], r=['protf'], w=['prot'])
        P.memset(self.ones32[:], 1.0, w=['ones32'])
        P.memset(self.onesb[:], 1.0, w=['onesb'])
        P.memset(self.bd64[:], 0.0, w=['bd64'])
        P.memset(self.bd64[0:64, 0:64], 1.0, w=['bd64'])
        P.memset(self.bd64[64:128, 64:128], 1.0, w=['bd64'])
        P.memset(P.epsc[:], EPS, w=['epsc'])
        self.staged = Ring(P, "wstg", [128, 1024], F32, 2)
        P.barrier()

    def load_w(self, dst, dkey, src, kc, F, scale_col=None, skey=None):
        P = self.P
        for c in range(kc):
            for f0 in range(0, F, 1024):
                fw = min(1024, F - f0)
                st, sk = self.staged.next()
                P.dma(st[:, 0:fw], src[c * 128:(c + 1) * 128, f0:f0 + fw], w=[sk])
                if scale_col is None:
                    P.copy(dst[:, c, f0:f0 + fw], st[:, 0:fw], r=[sk], w=[dkey], eng='pool')
                else:
                    P.ts(dst[:, c, f0:f0 + fw], st[:, 0:fw], scale_col[:, c:c + 1], ALU.mult, r=[sk, skey], w=[dkey])

    def rows_to_fm(self, rows, R, F, out, rkey, okey):
        P = self.P
        pb = self.pb[7]
        nchunk = F // 128
        for c in range(nchunk):
            P.tr(pb[:, c * R:(c + 1) * R], rows[0:R, c * 128:(c + 1) * 128], self.ident[0:R, 0:R],
                 r=[rkey, 'ident'], w=['pb7'])
        P.copy(out, pb[:, 0:nchunk * R].rearrange("p (c r) -> p c r", r=R), r=['pb7'], w=[okey])

    def phase_mod(self):
        P, io = self.P, self.io
        self.modfm = P.sb("modfm", [128, 48, 2], F32)
        self.Gb = P.sb("Gb", [128, 2, 2, D], F32)
        self.AB = P.sb("AB", [128, 4, 2, 8], F32)
        P.push()
        rows = P.sb("m_rows", [4, D], F32)
        sig = P.sb("m_sig", [2, D], F32)
        fm = P.sb("m_fm", [128, 8, 4], F32)
        sTb = P.sb("m_sTb", [128, 8, 2, 128], F32)
        modb = P.sb("m_modb", [1, 6 * D], F32)
        wr = Ring(P, "m_w", [128, 8, 512], F32, 2)
        P.dma(rows[0:2, :], io['cc'][:, :], w=['m_rows'])
        P.dma(rows[2:3, :], io['n1g'][:, :], w=['m_rows'])
        P.dma(rows[3:4, :], io['n2g'][:, :], w=['m_rows'])
        P.dma(modb[:], io['mod_b'][:, :], w=['m_modb'])
        P.act(sig[:], rows[0:2, :], AF.Sigmoid, r=['m_rows'], w=['m_sig'])
        P.tt(rows[0:2, :], rows[0:2, :], sig[:], ALU.mult, r=['m_rows', 'm_sig'], w=['m_rows'])
        self.rows_to_fm(rows, 4, D, fm[:], 'm_rows', 'm_fm')
        for c in range(8):
            for v in range(2):
                P.act(sTb[:, c, v, :], self.ones32[:], AF.Identity, scale=fm[:, c, v:v + 1],
                      r=['ones32', 'm_fm'], w=['m_sTb'])
        mw = io['mod_w'].rearrange("(c p) j -> p c j", p=128)
        for jb in range(12):
            piece, hf = jb // 2, jb % 2
            wt, wk = wr.next()
            P.dma(wt[:], mw[:, :, jb * 512:(jb + 1) * 512], w=[wk])
            pf = self.pb[0]
            for jj in range(4):
                j = jb * 4 + jj
                for c in range(8):
                    P.mm(pf[:, jj * 2:jj * 2 + 2], wt[:, c, jj * 128:(jj + 1) * 128], fm[:, c, 0:2],
                         start=(c == 0), stop=False, r=[wk, 'm_fm'], w=['pb0'])
                P.mm(pf[:, jj * 2:jj * 2 + 2], modb[0:1, j * 128:(j + 1) * 128], self.ones32[0:1, 0:2],
                     start=False, stop=True, r=['m_modb', 'ones32'], w=['pb0'])
            P.copy(self.modfm[:, jb * 4:(jb + 1) * 4, :], pf[:, 0:8].rearrange("p (j v) -> p j v", v=2),
                   r=['pb0'], w=['modfm'])
            if piece in (2, 5):
                gi = 0 if piece == 2 else 1
                for v in range(2):
                    pg = self.pb[1 + v]
                    for c in range(8):
                        P.mm(pg[:, :], sTb[:, c, v, :], wt[:, c, :], start=(c == 0), stop=False,
                             r=[wk, 'm_sTb'], w=[f'pb{1 + v}'])
                    P.mm(pg[:, :], self.ones32[0:1, 0:128], modb[0:1, jb * 512:(jb + 1) * 512],
                         start=False, stop=True, r=['m_modb', 'ones32'], w=[f'pb{1 + v}'])
                    P.copy(self.Gb[:, gi, v, hf * 512:(hf + 1) * 512], pg[:, :], r=[f'pb{1 + v}'], w=['Gb'], eng='act')
        for v in range(2):
            P.stt(self.AB[:, 0, v, :], self.modfm[:, 8:16, v], 1.0, fm[:, :, 2], ALU.add, ALU.mult,
                  r=['modfm', 'm_fm'], w=['AB'])
            P.copy(self.AB[:, 1, v, :], self.modfm[:, 0:8, v], r=['modfm'], w=['AB'])
            P.stt(self.AB[:, 2, v, :], self.modfm[:, 32:40, v], 1.0, fm[:, :, 3], ALU.add, ALU.mult,
                  r=['modfm', 'm_fm'], w=['AB'])
            P.copy(self.AB[:, 3, v, :], self.modfm[:, 24:32, v], r=['modfm'], w=['AB'])
        P.pop()

    def norm_tiles_to_hT(self, rowfn, t0, w, hT, hk, which, rings, isctx=None):
        P = self.P
        for ti in range(w // 128):
            a0 = t0 + ti * 128
            v = (1 if a0 < self.C else 0) if isctx is None else isctx
            xt, xk = rings['xt'].next()
            P.dma(xt[:], rowfn(a0), w=[xk])
            self.norm_one(xt, xk, v, hT, hk, ti, which, rings)

    def norm_one(self, xt, xk, v, hT, hk, ti, which, rings):
        P = self.P
        jk, jkk = rings['junk'].next()
        st, stk = rings['stat'].next()
        P.act(jk[:], xt[:], AF.Square, accum=st[:, 0:1], r=[xk], w=[jkk, stk])
        P.rsqrt(st[:, 1:2], st[:, 0:1], 1.0 / D, r=[stk], w=[stk])
        xn, xnk = rings['xn'].next()
        P.ts(xn[:], xt[:], st[:, 1:2], ALU.mult, r=[xk, stk], w=[xnk])
        pbi = rings['pbi']
        rings['pbi'] = 1 - pbi
        pt = self.pb[pbi][:].bitcast(BF16)
        for c in range(8):
            P.tr(pt[:, c * 128:(c + 1) * 128], xn[:, c * 128:(c + 1) * 128], self.identb[:],
                 r=[xnk, 'identb'], w=[f'pb{pbi}'])
        for c in range(8):
            P.act(hT[:, c, ti * 128:(ti + 1) * 128], pt[:, c * 128:(c + 1) * 128], AF.Identity,
                  scale=self.AB[:, which, v, c:c + 1], bias=self.AB[:, which + 1, v, c:c + 1],
                  r=[f'pb{pbi}', 'AB'], w=[hk])

    def norm_rings(self):
        P = self.P
        return {'xt': Ring(P, "n_xt", [128, D], F32, 2), 'junk': Ring(P, "n_junk", [128, D], BF16, 1),
                'stat': Ring(P, "n_stat", [128, 2], F32, 2), 'xn': Ring(P, "n_xn", [128, D], BF16, 2), 'pbi': 0}

    def phase_in_even(self):
        P, io = self.P, self.io
        NT, TQ = self.NT, self.Q1
        P.push()
        self.rT = P.sb("rT", [128, 4, NT], BF16)
        self.gT = P.sb("gT", [128, 4, self.Q1], BF16)
        P.push()
        win = P.sb("a_win", [128, 8, EVEN_IN], BF16)
        self.load_w(win, 'a_win', io['w_in'], 8, EVEN_IN)
        rings = self.norm_rings()
        hr = Ring(P, "a_hT", [128, 8, 512], BF16, 2)
        evr = Ring(P, "a_ev", [128, 512], BF16, 3)
        scr_idx = {'qc': 0, 'kvc': 3, 'kr': 5}
        chunks = [('g', j * 128, 128, j) for j in range(4)] + [('r', 512 + j * 128, 128, j) for j in range(4)] + \
                 [('qc', 1024 + j * 128, 128, j) for j in range(3)] + [('kvc', 1408 + j * 128, 128, j) for j in range(2)] + \
                 [('kr', 1664, 64, 0)]
        k = 0
        for gi, (t0, w) in enumerate(self.gA):
            hT, hk = hr.next()
            self.norm_tiles_to_hT(lambda a0: io['xa'][a0:a0 + 128, :], t0, w, hT, hk, 0, rings)
            inq = t0 < self.Q1
            for (nm, c0, cw, j) in chunks:
                if nm in ('g', 'qc') and not inq:
                    continue
                bi = 2 + (k % 4)
                k += 1
                ps = self.pb[bi]
                for c in range(8):
                    P.mm(ps[0:cw, 0:w], win[:, c, c0:c0 + cw], hT[:, c, 0:w], start=(c == 0), stop=(c == 7),
                         r=['a_win', hk], w=[f'pb{bi}'])
                if nm in ('g', 'r'):
                    dst = self.gT if nm == 'g' else self.rT
                    P.copy(dst[:, j, t0:t0 + w], ps[0:cw, 0:w], r=[f'pb{bi}'], w=[(nm, gi)], eng=('act' if k % 2 else 'dve'))
                else:
                    ev, evk = evr.next()
                    P.copy(ev[0:cw, 0:w], ps[0:cw, 0:w], r=[f'pb{bi}'], w=[evk], eng=('act' if k % 2 else 'dve'))
                    P.dma(io['qkvs'][scr_idx[nm] + j, 0:cw, t0:t0 + w], ev[0:cw, 0:w], r=[evk])
        P.pop()

    def phase_lru(self):
        P, io = self.P, self.io
        NT, C, Q1 = self.NT, self.C, self.Q1
        P.push()
        rows = P.sb("b_rows", [12, 512], F32)
        fm = P.sb("b_fm", [128, 4, 12], F32)
        cl = P.sb("b_cl", [128, 4, 2], F32)
        wbd = P.sb("b_wbd", [128, 16, 128], BF16)
        P.push()
        wst = P.sb("b_wst", [128, 16, 128], F32)
        P.dma(rows[0:5, :], io['conv5'][:, :], w=['b_rows'])
        P.dma(rows[5:6, :], io['conv_b'][:, :], w=['b_rows'])
        P.dma(rows[6:8, :], io['lru_ba'][:, :], w=['b_rows'])
        P.dma(rows[8:10, :], io['lru_bx'][:, :], w=['b_rows'])
        P.dma(rows[10:12, :], io['lru_lam'][:, :], w=['b_rows'])
        self.rows_to_fm(rows, 12, 512, fm[:], 'b_rows', 'b_fm')
        P.act(cl[:], fm[:, :, 10:12], AF.Exp, scale=-1.0, r=['b_fm'], w=['b_cl'])
        P.ts(cl[:], cl[:], 1.0, ALU.add, r=['b_cl'], w=['b_cl'])
        P.act(cl[:], cl[:], AF.Ln, r=['b_cl'], w=['b_cl'])
        P.ts(cl[:], cl[:], -8.0, ALU.mult, r=['b_cl'], w=['b_cl'])
        P.memset(wst[:], 0.0, w=['b_wst'])
        for d in range(2):
            for gt, nm in enumerate(('lru_wa', 'lru_wx')):
                for s in range(2):
                    i0 = (d * 2 + gt) * 4
                    srcw = io[nm][d].rearrange("(j s) k m -> s k j m", s=2)[s]
                    P.dma(wst[s * 64:(s + 1) * 64, i0:i0 + 4, s * 64:(s + 1) * 64], srcw, w=['b_wst'])
        P.copy(wbd[:], wst[:], r=['b_wst'], w=['b_wbd'])
        ta = Ring(P, "b_ta", [128, 512], F32, 2)
        tb = Ring(P, "b_tb", [128, 512], F32, 2)
        for gi, (t0, w) in enumerate(groups(0, Q1)):
            for j in range(4):
                g_ap = self.gT[:, j, t0:t0 + w]
                a, ak = ta.next()
                b, bk = tb.next()
                gk = ('gT', j, gi)
                P.tt(a[:, 0:w], g_ap, g_ap, ALU.mult, r=[gk], w=[ak])
                P.ts(a[:, 0:w], a[:, 0:w], 0.044715, ALU.mult, 1.0, ALU.add, r=[ak], w=[ak])
                P.tt(a[:, 0:w], a[:, 0:w], g_ap, ALU.mult, r=[ak, gk], w=[ak])
                P.act(b[:, 0:w], a[:, 0:w], AF.Sigmoid, scale=1.5957691216057308, r=[ak], w=[bk])
                P.tt(g_ap, b[:, 0:w], g_ap, ALU.mult, r=[bk, gk], w=[gk])
        P.pop()
        xcv = P.sb("b_xcv", [128, NT], F32)
        xcvb = P.sb("b_xcvb", [128, NT], BF16)
        recF = P.sb("b_recF", [128, 4, Q1], BF16)
        tr_ = Ring(P, "b_rg", [128, 512], F32, 2)
        ti_ = Ring(P, "b_ig", [128, 512], F32, 2)
        taa = Ring(P, "b_aa", [128, 512], F32, 2)
        tm_ = Ring(P, "b_mm", [128, 512], F32, 2)
        tu_ = Ring(P, "b_uu", [128, 512], F32, 2)
        ths = Ring(P, "b_hs", [128, 512], F32, 2)
        seqs = [(0, C), (C, NT)]
        blocks_f = list(self.gA)
        blocks_b = [g for g in self.gA if g[0] < C][::-1] + [g for g in self.gA if g[0] >= C][::-1]
        kk = 0
        for j in range(4):
            for (s0, s1) in seqs:
                P.ts(xcv[:, s0:s1], self.rT[:, j, s0:s1], fm[:, j, 2:3], ALU.mult, fm[:, j, 5:6], ALU.add,
                     r=['b_fm'], w=['b_xcv'])
                for o in (-2, -1, 1, 2):
                    d0, d1 = max(s0, s0 - o), min(s1, s1 - o)
                    P.stt(xcv[:, d0:d1], self.rT[:, j, d0 + o:d1 + o], fm[:, j, o + 2:o + 3], xcv[:, d0:d1],
                          ALU.mult, ALU.add, r=['b_fm', 'b_xcv'], w=['b_xcv'])
            P.copy(xcvb[:], xcv[:], r=['b_xcv'], w=['b_xcvb'], eng='act')
            for d in range(2):
                prev = None
                for (t0, w) in (blocks_f if d == 0 else blocks_b):
                    kk += 1
                    b0, b1 = kk % 2, 2 + kk % 2
                    P.mm(self.pb[b0][:, 0:w], wbd[:, (d * 2 + 0) * 4 + j, :], xcvb[:, t0:t0 + w],
                         r=['b_wbd', 'b_xcvb'], w=[f'pb{b0}'])
                    P.mm(self.pb[b1][:, 0:w], wbd[:, (d * 2 + 1) * 4 + j, :], xcvb[:, t0:t0 + w],
                         r=['b_wbd', 'b_xcvb'], w=[f'pb{b1}'])
                    rg, rgk = tr_.next()
                    ig, igk = ti_.next()
                    aa, aak = taa.next()
                    mm_, mk = tm_.next()
                    uu, uk = tu_.next()
                    hs, hk = ths.next()
                    P.act(rg[:, 0:w], self.pb[b0][:, 0:w], AF.Sigmoid, bias=fm[:, j, 6 + d:7 + d], r=[f'pb{b0}', 'b_fm'], w=[rgk])
                    P.act(ig[:, 0:w], self.pb[b1][:, 0:w], AF.Sigmoid, bias=fm[:, j, 8 + d:9 + d], r=[f'pb{b1}', 'b_fm'], w=[igk])
                    P.act(aa[:, 0:w], rg[:, 0:w], AF.Exp, scale=cl[:, j, d:d + 1], r=[rgk, 'b_cl'], w=[aak])
                    P.tt(mm_[:, 0:w], aa[:, 0:w], aa[:, 0:w], ALU.mult, r=[aak], w=[mk], eng='pool')
                    P.ts(mm_[:, 0:w], mm_[:, 0:w], -1.0, ALU.mult, 1.0, ALU.add, r=[mk], w=[mk], eng='pool')
                    P.act(mm_[:, 0:w], mm_[:, 0:w], AF.Sqrt, r=[mk], w=[mk])
                    P.tt(uu[:, 0:w], mm_[:, 0:w], ig[:, 0:w], ALU.mult, r=[mk, igk], w=[uk], eng='pool')
                    P.tt(uu[:, 0:w], uu[:, 0:w], xcv[:, t0:t0 + w], ALU.mult, r=[uk, 'b_xcv'], w=[uk], eng='pool')
                    init = 0.0 if prev is None else prev[0][:, prev[2] - 1:prev[2]]
                    rk = [aak, uk] + ([] if prev is None else [prev[1]])
                    if d == 0:
                        P.scan(hs[:, 0:w], aa[:, 0:w], uu[:, 0:w], init, r=rk, w=[hk])
                    else:
                        P.scan(hs[:, 0:w], aa[:, 0:w][:, ::-1], uu[:, 0:w][:, ::-1], init, r=rk, w=[hk])
                    prev = (hs, hk, w)
                    if t0 < Q1:
                        if d == 0:
                            P.copy(recF[:, j, t0:t0 + w], hs[:, 0:w], r=[hk], w=[('recF', j, t0)], eng='act')
                        else:
                            P.tt(mm_[:, 0:w], hs[:, 0:w][:, ::-1], recF[:, j, t0:t0 + w], ALU.add,
                                 r=[hk, ('recF', j, t0)], w=[mk])
                            P.tt(self.gT[:, j, t0:t0 + w], mm_[:, 0:w], self.gT[:, j, t0:t0 + w], ALU.mult,
                                 r=[mk], w=[('gTo', j, t0)])
        P.barrier()
        for j in range(4):
            P.dma(io['mix'][j, :, 0:Q1], self.gT[:, j, :])
        P.pop()
        P.pop()
        P.push()
        self.qcT = P.sb("qcT", [128, 3, self.Q1], BF16)
        self.kvcT = P.sb("kvcT", [128, 2, NT], BF16)
        self.krT = P.sb("krT", [64, NT], BF16)
        for j in range(3):
            P.dma(self.qcT[:, j, :], io['qkvs'][j, :, 0:self.Q1])
        for j in range(2):
            P.dma(self.kvcT[:, j, :], io['qkvs'][3 + j, :, :])
        P.dma(self.krT[:, :], io['qkvs'][5, 0:64, :])
        P.barrier()

    def rope_fm(self, dst, src_bf, np_, w, pos0, tmpr, skey, dkey, pbi, tabs=('rcos', 'rsin')):
        P = self.P
        ps = self.pb[pbi]
        P.mm(ps[0:np_, 0:w], self.prot[0:np_, 0:np_], src_bf, r=['prot', skey], w=[f'pb{pbi}'])
        t1, t1k = tmpr.next()
        t2, t2k = tmpr.next()
        cs, csk = self.ropr.next()
        sn, snk = self.ropr.next()
        P.dma(cs[0:np_, 0:w], self.io[tabs[0]][0:np_, pos0:pos0 + w], w=[csk])
        P.dma(sn[0:np_, 0:w], self.io[tabs[1]][0:np_, pos0:pos0 + w], w=[snk])
        P.tt(t1[0:np_, 0:w], src_bf, cs[0:np_, 0:w], ALU.mult, r=[skey, csk], w=[t1k], eng='pool')
        P.tt(t2[0:np_, 0:w], ps[0:np_, 0:w], sn[0:np_, 0:w], ALU.mult, r=[f'pb{pbi}', snk], w=[t2k])
        P.tt(dst, t1[0:np_, 0:w], t2[0:np_, 0:w], ALU.add, r=[t1k, t2k], w=[dkey])

    def load_rope(self):
        P, io = self.P, self.io
        self.rcos = P.sb("rcos", [128, self.S], F32)
        self.rsin = P.sb("rsin", [128, self.S], F32)
        P.dma(self.rcos[:], io['rcos'][:, :], w=['rope'])
        P.dma(self.rsin[:], io['rsin'][:, :], w=['rope'])

    def phase_mla(self):
        P, io = self.P, self.io
        NT, C, Q1 = self.NT, self.C, self.Q1
        pb = self.pb
        P.push()
        self.ropr = Ring(P, "ropr", [128, 512], F32, 4)
        aor = Ring(P, "c_ao", [128, 512], BF16, 2)
        rows = P.sb("c_rows", [4, 384], F32)
        fm = P.sb("c_fm", [128, 3, 4], F32)
        P.memset(rows[:], 0.0, w=['c_rows'])
        P.dma(rows[0:1, 0:384], io['q_norm_g'][:, :], w=['c_rows'])
        P.dma(rows[1:2, 0:256], io['kv_norm_g'][:, :], w=['c_rows'])
        P.dma(rows[2:3, 0:192], io['qn_g'][:, :], w=['c_rows'])
        P.dma(rows[3:4, 0:192], io['kn_g'][:, :], w=['c_rows'])
        self.rows_to_fm(rows, 4, 384, fm[:], 'c_rows', 'c_fm')
        gq = P.sb("c_gq", [128, 3], F32)
        gkv = P.sb("c_gkv", [128, 2], F32)
        P.copy(gq[:], fm[:, :, 0], r=['c_fm'], w=['c_gq'])
        P.copy(gkv[:], fm[:, 0:2, 1], r=['c_fm'], w=['c_gkv'])
        wq = P.sb("c_wq", [128, 3, 768], BF16)
        wkv = P.sb("c_wkv", [128, 2, 1024], BF16)
        self.load_w(wq, 'c_wq', io['w_uq'], 3, 768, scale_col=gq, skey='c_gq')
        self.load_w(wkv, 'c_wkv', io['w_ukv'], 2, 1024, scale_col=gkv, skey='c_gkv')
        qng_n, qng_r = fm[:, 0, 2:3], fm[0:64, 1, 2:3]
        kng_n, kng_r = fm[:, 0, 3:4], fm[0:64, 1, 3:4]
        knT = P.sb("c_knT", [128, NT], BF16)
        krh = P.sb("c_krh", [64, NT], BF16)
        vh = P.sb("c_vh", [128, NT // 128, 128], BF16)
        qnT = P.sb("c_qnT", [128, Q1], BF16)
        qrh = P.sb("c_qrh", [64, Q1], BF16)
        sqkv = Ring(P, "c_sqkv", [128, 2, 512], BF16, 2)
        sq3 = Ring(P, "c_sq3", [128, 3, 512], BF16, 2)
        sqb = Ring(P, "c_sqb", [128, 512], BF16, 3)
        f1 = Ring(P, "c_f", [128, 512], F32, 8)
        small = Ring(P, "c_small", [128, 2], F32, 4)
        pT = Ring(P, "c_pT", [128, 512], BF16, 3)
        SC = float(192 ** -0.5)
        for h in range(MH):
            P.barrier()
            for gi, (t0, w) in enumerate(self.gA):
                sk, skk = sqkv.next()
                for c in range(2):
                    P.act(sk[:, c, 0:w], self.kvcT[:, c, t0:t0 + w], AF.Square, w=[skk])
                for c in range(2):
                    P.mm(pb[0][:, 0:w], self.onesb[:], sk[:, c, 0:w], start=(c == 0), stop=(c == 1), r=['onesb', skk], w=['pb0'])
                rkv2, rkv2k = f1.next()
                P.ts(rkv2[:, 0:w], pb[0][:, 0:w], 1.0 / KVR, ALU.mult, EPS, ALU.add, r=['pb0'], w=[rkv2k])
                P.recip(rkv2[:, 0:w], rkv2[:, 0:w], r=[rkv2k], w=[rkv2k])
                rkv, rkvk = f1.next()
                P.act(rkv[:, 0:w], rkv2[:, 0:w], AF.Sqrt, r=[rkv2k], w=[rkvk])
                s2, s2k = sqb.next()
                P.act(s2[0:64, 0:w], self.krT[0:64, t0:t0 + w], AF.Square, w=[s2k])
                P.mm(pb[1][:, 0:w], self.onesb[0:64, :], s2[0:64, 0:w], r=['onesb', s2k], w=['pb1'])
                sskr, sskrk = f1.next()
                P.copy(sskr[:, 0:w], pb[1][:, 0:w], r=['pb1'], w=[sskrk], eng='act')
                for c in range(2):
                    P.mm(pb[2][:, 0:w], wkv[:, c, 256 * h:256 * h + 128], self.kvcT[:, c, t0:t0 + w],
                         start=(c == 0), stop=(c == 1), r=['c_wkv'], w=['pb2'])
                s3, s3k = sqb.next()
                P.act(s3[:, 0:w], pb[2][:, 0:w], AF.Square, r=['pb2'], w=[s3k])
                P.mm(pb[3][:, 0:w], self.onesb[:], s3[:, 0:w], r=['onesb', s3k], w=['pb3'])
                kt, ktk = f1.next()
                P.tt(kt[:, 0:w], pb[3][:, 0:w], rkv2[:, 0:w], ALU.mult, r=['pb3', rkv2k], w=[ktk])
                P.tt(kt[:, 0:w], kt[:, 0:w], sskr[:, 0:w], ALU.add, r=[ktk, sskrk], w=[ktk])
                P.rsqrt(kt[:, 0:w], kt[:, 0:w], 1.0 / 192, r=[ktk], w=[ktk])
                cb, cbk = f1.next()
                P.tt(cb[:, 0:w], kt[:, 0:w], rkv[:, 0:w], ALU.mult, r=[ktk, rkvk], w=[cbk])
                P.stt(knT[:, t0:t0 + w], pb[2][:, 0:w], kng_n, cb[:, 0:w], ALU.mult, ALU.mult,
                      r=['pb2', cbk, 'c_fm'], w=[('knT', gi)])
                if t0 < C:
                    P.stt(krh[0:64, t0:t0 + w], self.krT[0:64, t0:t0 + w], kng_r, kt[0:64, 0:w], ALU.mult, ALU.mult,
                          r=[ktk, 'c_fm'], w=[('krh', gi)])
                else:
                    kn_, knk = sqb.next()
                    P.stt(kn_[0:64, 0:w], self.krT[0:64, t0:t0 + w], kng_r, kt[0:64, 0:w], ALU.mult, ALU.mult,
                          r=[ktk, 'c_fm'], w=[knk])
                    self.rope_fm(krh[0:64, t0:t0 + w], kn_[0:64, 0:w], 64, w, t0 - C, f1, knk, ('krh', gi), 1)
                for ti in range(w // 128):
                    a0 = t0 + ti * 128
                    pv = pb[4 + (ti % 2)]
                    for c in range(2):
                        P.mm(pv[:, 0:128], self.kvcT[:, c, a0:a0 + 128], wkv[:, c, 256 * h + 128:256 * h + 256],
                             start=(c == 0), stop=(c == 1), r=['c_wkv'], w=[f'pb{4 + ti % 2}'])
                    for c in range(2):
                        P.mm(pb[6][:, 0:2], sk[:, c, ti * 128:(ti + 1) * 128], self.onesb[:, 0:2],
                             start=(c == 0), stop=(c == 1), r=['onesb', skk], w=['pb6'])
                    sm, smk = small.next()
                    P.rsqrt(sm[:, 0:1], pb[6][:, 0:1], 1.0 / KVR, r=['pb6'], w=[smk])
                    P.ts(vh[:, a0 // 128, :], pv[:, 0:128], sm[:, 0:1], ALU.mult, r=[f'pb{4 + ti % 2}', smk], w=[('vh', a0)])
            for gi, (t0, w) in enumerate(self.gQ):
                s3_, s3k_ = sq3.next()
                for c in range(3):
                    P.act(s3_[:, c, 0:w], self.qcT[:, c, t0:t0 + w], AF.Square, w=[s3k_])
                for c in range(3):
                    P.mm(pb[0][:, 0:w], self.onesb[:], s3_[:, c, 0:w], start=(c == 0), stop=(c == 2), r=['onesb', s3k_], w=['pb0'])
                eq, eqk = f1.next()
                P.ts(eq[:, 0:w], pb[0][:, 0:w], EPS / QR, ALU.mult, EPS * EPS, ALU.add, r=['pb0'], w=[eqk])
                for c in range(3):
                    P.mm(pb[2][:, 0:w], wq[:, c, 192 * h:192 * h + 128], self.qcT[:, c, t0:t0 + w],
                         start=(c == 0), stop=(c == 2), r=['c_wq'], w=['pb2'])
                for c in range(3):
                    P.mm(pb[3][0:64, 0:w], wq[:, c, 192 * h + 128:192 * h + 192], self.qcT[:, c, t0:t0 + w],
                         start=(c == 0), stop=(c == 2), r=['c_wq'], w=['pb3'])
                sa, sak = sqb.next()
                sb_, sbk = sqb.next()
                P.act(sa[:, 0:w], pb[2][:, 0:w], AF.Square, r=['pb2'], w=[sak])
                P.act(sb_[0:64, 0:w], pb[3][0:64, 0:w], AF.Square, r=['pb3'], w=[sbk])
                P.mm(pb[1][:, 0:w], self.onesb[:], sa[:, 0:w], start=True, stop=False, r=['onesb', sak], w=['pb1'])
                P.mm(pb[1][:, 0:w], self.onesb[0:64, :], sb_[0:64, 0:w], start=False, stop=True, r=['onesb', sbk], w=['pb1'])
                qs, qsk = f1.next()
                P.stt(qs[:, 0:w], pb[1][:, 0:w], 1.0 / 192, eq[:, 0:w], ALU.mult, ALU.add, r=['pb1', eqk], w=[qsk])
                P.act(qs[:, 0:w], qs[:, 0:w], AF.Sqrt, r=[qsk], w=[qsk])
                P.recip(qs[:, 0:w], qs[:, 0:w], r=[qsk], w=[qsk])
                P.stt(qnT[:, t0:t0 + w], pb[2][:, 0:w], qng_n, qs[:, 0:w], ALU.mult, ALU.mult, r=['pb2', qsk, 'c_fm'], w=[('qnT', gi)])
                if t0 < C:
                    P.stt(qrh[0:64, t0:t0 + w], pb[3][0:64, 0:w], qng_r, qs[0:64, 0:w], ALU.mult, ALU.mult,
                          r=['pb3', qsk, 'c_fm'], w=[('qrh', gi)])
                else:
                    qn_, qnk = sqb.next()
                    P.stt(qn_[0:64, 0:w], pb[3][0:64, 0:w], qng_r, qs[0:64, 0:w], ALU.mult, ALU.mult,
                          r=['pb3', qsk, 'c_fm'], w=[qnk])
                    self.rope_fm(qrh[0:64, t0:t0 + w], qn_[0:64, 0:w], 64, w, t0 - C, f1, qnk, ('qrh', gi), 0)
            P.barrier()
            for qi, (t0, w) in enumerate(self.gQ):
                nkt = (C // 128) if t0 < C else (NT // 128)
                po, pl = pb[4 + 2 * (qi % 2)], pb[5 + 2 * (qi % 2)]
                pok, plk = f'pb{4 + 2 * (qi % 2)}', f'pb{5 + 2 * (qi % 2)}'
                for kt_ in range(nkt):
                    k0 = kt_ * 128
                    bi = kt_ % 4
                    P.mm(pb[bi][:, 0:w], knT[:, k0:k0 + 128], qnT[:, t0:t0 + w], start=True, stop=False, w=[f'pb{bi}'])
                    P.mm(pb[bi][:, 0:w], krh[0:64, k0:k0 + 128], qrh[0:64, t0:t0 + w], start=False, stop=True, w=[f'pb{bi}'])
                    p_, pk = pT.next()
                    P.act(p_[:, 0:w], pb[bi][:, 0:w], AF.Exp, scale=SC, r=[f'pb{bi}'], w=[pk])
                    P.mm(po[:, 0:w], vh[:, kt_, :], p_[:, 0:w], start=(kt_ == 0), stop=(kt_ == nkt - 1), r=[pk], w=[pok])
                    P.mm(pl[:, 0:w], self.onesb[:], p_[:, 0:w], start=(kt_ == 0), stop=(kt_ == nkt - 1), r=[pk], w=[plk])
                rl, rlk = f1.next()
                P.recip(rl[:, 0:w], pl[:, 0:w], r=[plk], w=[rlk])
                ao, aok = aor.next()
                P.tt(ao[:, 0:w], po[:, 0:w], rl[:, 0:w], ALU.mult, r=[pok, rlk], w=[aok])
                P.dma(io['mix'][4 + h, :, t0:t0 + w], ao[:, 0:w], r=[aok])
        P.pop()
        P.pop()

    def phase_out(self, w_out_name):
        P, io, pb = self.P, self.io, self.pb
        Q0, Q1, TQ, NE, C = self.Q0, self.Q1, self.TQ, self.NE, self.C
        P.push()
        mixr = Ring(P, "d_mix", [128, 8, 128], BF16, 2)
        h2r = Ring(P, "d_h2", [128, 8, 128], BF16, 2)
        wout = P.sb("d_wout", [128, 8, D], BF16)
        wr = P.sb("d_wr", [128, 8, NE], F32)
        rb = P.sb("d_rb", [1, NE], F32)
        P.dma(wr[:], io['router_w'].rearrange("(c p) e -> p c e", p=128), w=['d_wr'])
        P.dma(rb[:], io['router_b'][:, :], w=['d_rb'])
        self.load_w(wout, 'd_wout', io[w_out_name], 8, D)
        xr = Ring(P, "d_xt", [128, D], F32, 2)
        x1r = Ring(P, "d_x1", [128, D], F32, 2)
        xnr = Ring(P, "d_xn", [128, D], F32, 2)
        jr = Ring(P, "d_junk", [128, D], BF16, 1)
        str_ = Ring(P, "d_stat", [128, 2], F32, 2)
        h32r = Ring(P, "d_h32", [128, 8, 128], F32, 2)
        lgr = Ring(P, "d_lg", [128, 4, NE], F32, 2)
        t8r = Ring(P, "d_t8", [128, 16], F32, 2)
        gtr = Ring(P, "d_gt", [NE, 128], F32, 2)
        for ti in range(TQ // 128):
            q0 = ti * 128
            a0 = Q0 + q0
            v = 1 if a0 < C else 0
            pa, pbk = (0, 1) if ti % 2 == 0 else (2, 3)
            mx, mxk = mixr.next()
            P.dma(mx[:], io['mix'][:, :, q0:q0 + 128].rearrange("c p t -> p c t"), w=[mxk])
            for hf, bi in enumerate((pa, pbk)):
                for c in range(8):
                    P.mm(pb[bi][:, :], mx[:, c, :], wout[:, c, hf * 512:(hf + 1) * 512],
                         start=(c == 0), stop=(c == 7), r=[mxk, 'd_wout'], w=[f'pb{bi}'])
            xt, xk = xr.next()
            P.dma(xt[:], self.xrow(a0), w=[xk])
            x1, x1k = x1r.next()
            for hf, bi in enumerate((pa, pbk)):
                P.tt(x1[:, hf * 512:(hf + 1) * 512], pb[bi][:, :], self.Gb[:, 0, v, hf * 512:(hf + 1) * 512], ALU.mult,
                     r=[f'pb{bi}', 'Gb'], w=[x1k])
            P.tt(x1[:], x1[:], xt[:], ALU.add, r=[x1k, xk], w=[x1k], eng='pool')
            P.dma(io['x1s'][q0:q0 + 128, :], x1[:], r=[x1k])
            jk, jkk = jr.next()
            st, stk = str_.next()
            P.act(jk[:], x1[:], AF.Square, accum=st[:, 0:1], r=[x1k], w=[jkk, stk])
            P.rsqrt(st[:, 1:2], st[:, 0:1], 1.0 / D, r=[stk], w=[stk])
            xn, xnk = xnr.next()
            P.ts(xn[:], x1[:], st[:, 1:2], ALU.mult, r=[x1k, stk], w=[xnk])
            for c in range(8):
                bi = 4 + c // 4
                P.tr(pb[bi][:, (c % 4) * 128:(c % 4 + 1) * 128], xn[:, c * 128:(c + 1) * 128], self.ident[:],
                     r=[xnk, 'ident'], w=[f'pb{bi}'])
            h32, hk = h32r.next()
            for c in range(8):
                bi = 4 + c // 4
                P.act(h32[:, c, :], pb[bi][:, (c % 4) * 128:(c % 4 + 1) * 128], AF.Identity,
                      scale=self.AB[:, 2, v, c:c + 1], bias=self.AB[:, 3, v, c:c + 1], r=[f'pb{bi}', 'AB'], w=[hk])
            h2, h2k = h2r.next()
            P.copy(h2[:], h32[:], r=[hk], w=[h2k], eng='pool')
            P.dma(io['h2s'][:, :, q0:q0 + 128].rearrange("c p t -> p c t"), h2[:], r=[h2k])
            for c in range(8):
                P.mm(pb[6][:, 0:NE], h32[:, c, :], wr[:, c, :], start=(c == 0), stop=False, r=[hk, 'd_wr'], w=['pb6'])
            P.mm(pb[6][:, 0:NE], self.ones32[0:1, 0:128], rb[0:1, :], start=False, stop=True, r=['ones32', 'd_rb'], w=['pb6'])
            lg, lgk = lgr.next()
            t8, t8k = t8r.next()
            P.copy(lg[:, 0, :], pb[6][:, 0:NE], r=['pb6'], w=[lgk])
            P.op('dve', lambda e, o=t8[:, 0:8], i=lg[:, 0, :]: e.max(out=o, in_=i), r=[lgk], w=[t8k])
            P.ts(lg[:, 1, :], lg[:, 0, :], t8[:, TOPK - 1:TOPK], ALU.is_ge, r=[lgk, t8k], w=[lgk])
            P.ts(t8[:, 8:9], t8[:, 0:1], -1.0, ALU.mult, r=[t8k], w=[t8k])
            P.act(lg[:, 2, :], lg[:, 0, :], AF.Exp, bias=t8[:, 8:9], r=[lgk, t8k], w=[lgk])
            P.tt(lg[:, 2, :], lg[:, 2, :], lg[:, 1, :], ALU.mult, r=[lgk], w=[lgk])
            P.op('dve', lambda e, o=t8[:, 9:10], i=lg[:, 2, :]: e.reduce_sum(out=o, in_=i, axis=AX.X), r=[lgk, t8k], w=[t8k])
            P.recip(t8[:, 10:11], t8[:, 9:10], r=[t8k], w=[t8k])
            P.ts(lg[:, 3, :], lg[:, 2, :], t8[:, 10:11], ALU.mult, r=[lgk, t8k], w=[lgk])
            P.tr(pb[7][0:NE, 0:128], lg[:, 3, :], self.ident[:], r=[lgk, 'ident'], w=['pb7'])
            gt, gtk = gtr.next()
            P.copy(gt[:], pb[7][0:NE, 0:128], r=['pb7'], w=[gtk], eng='act')
            P.dma(io['gates'][:, q0:q0 + 128], gt[:], r=[gtk])
        P.pop()

    def phase_moe(self, out_rows):
        P, io, pb = self.P, self.io, self.pb
        Q0, TQ, NE, C = self.Q0, self.TQ, self.NE, self.C
        P.push()
        ntile = TQ // 128
        npass = -(-ntile // 9) if ntile > 2 else ntile
        base, extra = ntile // npass, ntile % npass
        psizes = [(base + (1 if i < extra else 0)) * 128 for i in range(npass)]
        pstarts = [sum(psizes[:i]) for i in range(npass)]
        acc = P.sb("e_acc", [128, 8, max(psizes)], F32)
        hl2 = P.sb("e_hl2", [128, 8, max(psizes)], BF16)
        brow = P.sb("e_brow", [NE, 3 * FF], F32)
        bfm = P.sb("e_bfm", [128, 16, NE], F32)
        bd32 = brow[0:NE, 2 * FF:3 * FF]
        P.dma(brow[0:NE, 0:2 * FF], io['b_gu'][:, :], w=['e_brow'])
        P.dma(bd32, io['b_down'][:, :], w=['e_brow'])
        self.rows_to_fm(brow, NE, 2 * FF, bfm[:], 'e_brow', 'e_bfm')
        wgr = Ring(P, "e_wg", [128, 8, 1024], BF16, 2)
        wdr = Ring(P, "e_wd", [128, 4, D], BF16, 2)
        gbr = Ring(P, "e_gb", [128, 512], F32, 2)
        gtr = Ring(P, "e_gt", [NE, 512], F32, 2)
        e1 = Ring(P, "e_gcl", [128, 512], F32, 2)
        e2 = Ring(P, "e_sig", [128, 512], F32, 2)
        e3 = Ring(P, "e_ucl", [128, 512], F32, 2)
        agr = Ring(P, "e_actg", [128, 4, 512], BF16, 2)
        xr = Ring(P, "e_x1", [128, D], F32, 2)
        orr = Ring(P, "e_o", [128, D], F32, 2)
        kq = 0
        for tp in range(npass):
            p0, TP = pstarts[tp], psizes[tp]
            ngr = -(-TP // 512)
            grp = groups(p0, p0 + TP, -(-(TP // 128) // ngr) * 128)
            for c in range(8):
                P.dma(hl2[:, c, 0:TP], io['h2s'][c, :, p0:p0 + TP], w=['e_hl2'])
            for (t0, w) in grp:
                gt, gtk = gtr.next()
                P.dma(gt[:, 0:w], io['gates'][:, t0:t0 + w], w=[gtk])
                for dc in range(8):
                    bi = dc % 4
                    P.mm(pb[bi][:, 0:w], bd32[:, dc * 128:(dc + 1) * 128], gt[:, 0:w], r=['e_brow', gtk], w=[f'pb{bi}'])
                    P.copy(acc[:, dc, t0 - p0:t0 - p0 + w], pb[bi][:, 0:w], r=[f'pb{bi}'], w=[('acc', dc, t0)],
                           eng=('act' if dc % 2 else 'dve'))
            hes = [(e, hf) for e in range(NE) for hf in range(2)]

            def load_he(e, hf):
                wg, wgk = wgr.next()
                wd, wdk = wdr.next()
                for c in range(8):
                    for part in range(2):
                        st, sk = self.staged.next()
                        col0 = part * FF + hf * 512
                        P.dma(st[:, 0:512], io['w_gu'][e, c * 128:(c + 1) * 128, col0:col0 + 512], w=[sk])
                        P.copy(wg[:, c, part * 512:(part + 1) * 512], st[:, 0:512], r=[sk], w=[wgk], eng='pool')
                for c in range(4):
                    st, sk = self.staged.next()
                    r0 = hf * 512 + c * 128
                    P.dma(st[:, 0:D], io['w_down'][e, r0:r0 + 128, :], w=[sk])
                    P.copy(wd[:, c, :], st[:, 0:D], r=[sk], w=[wdk], eng='pool')
                return wg, wgk, wd, wdk

            def down(pend):
                ag, agk, t0, w, wd, wdk = pend
                for dc in range(8):
                    bi = 4 + dc % 4
                    for fc in range(4):
                        P.mm(pb[bi][:, 0:w], wd[:, fc, dc * 128:(dc + 1) * 128], ag[:, fc, 0:w],
                             start=(fc == 0), stop=(fc == 3), r=[wdk, agk], w=[f'pb{bi}'])
                    a_ap = acc[:, dc, t0 - p0:t0 - p0 + w]
                    P.tt(a_ap, pb[bi][:, 0:w], a_ap, ALU.add, r=[f'pb{bi}', ('acc', dc, t0)], w=[('acc', dc, t0)])

            nxt = load_he(*hes[0])
            pending = None
            for i, (e, hf) in enumerate(hes):
                wg, wgk, wd, wdk = nxt
                for gidx, (t0, w) in enumerate(grp):
                    gb, gbk = gbr.next()
                    P.dma(gb[:, 0:w], io['gates'][e:e + 1, t0:t0 + w].partition_broadcast(128), w=[gbk])
                    ag, agk = agr.next()
                    for fc in range(4):
                        kq += 1
                        bg, bu = (kq % 2) * 2, (kq % 2) * 2 + 1
                        for c in range(8):
                            P.mm(pb[bg][:, 0:w], wg[:, c, fc * 128:(fc + 1) * 128], hl2[:, c, t0 - p0:t0 - p0 + w],
                                 start=(c == 0), stop=(c == 7), r=[wgk, 'e_hl2'], w=[f'pb{bg}'])
                        for c in range(8):
                            P.mm(pb[bu][:, 0:w], wg[:, c, 512 + fc * 128:512 + (fc + 1) * 128], hl2[:, c, t0 - p0:t0 - p0 + w],
                                 start=(c == 0), stop=(c == 7), r=[wgk, 'e_hl2'], w=[f'pb{bu}'])
                        fg = hf * 4 + fc
                        gcl, gck = e1.next()
                        sg, sgk = e2.next()
                        ucl, uck = e3.next()
                        P.ts(gcl[:, 0:w], pb[bg][:, 0:w], bfm[:, fg, e:e + 1], ALU.add, LIMIT, ALU.min,
                             r=[f'pb{bg}', 'e_bfm'], w=[gck])
                        P.act(sg[:, 0:w], gcl[:, 0:w], AF.Sigmoid, scale=ALPHA, r=[gck], w=[sgk])
                        P.ts(ucl[:, 0:w], pb[bu][:, 0:w], bfm[:, 8 + fg, e:e + 1], ALU.add, LIMIT, ALU.min,
                             r=[f'pb{bu}', 'e_bfm'], w=[uck])
                        P.ts(ucl[:, 0:w], ucl[:, 0:w], -LIMIT, ALU.max, 1.0, ALU.add, r=[uck], w=[uck])
                        P.tt(ucl[:, 0:w], ucl[:, 0:w], gb[:, 0:w], ALU.mult, r=[uck, gbk], w=[uck])
                        P.tt(gcl[:, 0:w], gcl[:, 0:w], sg[:, 0:w], ALU.mult, r=[gck, sgk], w=[gck])
                        P.tt(ag[:, fc, 0:w], gcl[:, 0:w], ucl[:, 0:w], ALU.mult, r=[gck, uck], w=[agk])
                    if pending is not None:
                        down(pending)
                    pending = (ag, agk, t0, w, wd, wdk)
                    if gidx == 0 and i + 1 < len(hes):
                        nxt = load_he(*hes[i + 1])
            down(pending)
            for ti in range(TP // 128):
                q0 = p0 + ti * 128
                a0 = Q0 + q0
                v = 1 if a0 < C else 0
                for c in range(8):
                    bi = (ti % 2) * 2 + c // 4
                    keys = [('acc', c, t0) for (t0, w) in grp if t0 <= q0 < t0 + w]
                    P.tr(pb[bi][:, (c % 4) * 128:(c % 4 + 1) * 128], acc[:, c, ti * 128:(ti + 1) * 128], self.ident[:],
                         r=keys + ['ident'], w=[f'pb{bi}'])
                x1, x1k = xr.next()
                P.dma(x1[:], io['x1s'][q0:q0 + 128, :], w=[x1k])
                o, ok_ = orr.next()
                for hf in range(2):
                    bi = (ti % 2) * 2 + hf
                    P.tt(o[:, hf * 512:(hf + 1) * 512], pb[bi][:, :], self.Gb[:, 1, v, hf * 512:(hf + 1) * 512], ALU.mult,
                         r=[f'pb{bi}', 'Gb'], w=[ok_])
                P.tt(o[:], o[:], x1[:], ALU.add, r=[ok_, x1k], w=[ok_], eng='pool')
                for (dst, qs, n) in out_rows:
                    if qs <= q0 < qs + n:
                        P.dma(dst[q0 - qs:q0 - qs + 128, :], o[:], r=[ok_])
            P.barrier()
        P.pop()

    def phase_diff(self):
        import math
        P, io, pb = self.P, self.io, self.pb
        NT, C, OWN = self.NT, self.C, self.OWN
        lam_init = 0.8 - 0.6 * math.exp(-0.3 * self.layer)
        gw = min(512, OWN)
        qgroups = groups(0, OWN, gw)
        P.push()
        hT = P.sb("f_hT", [128, 8, NT], BF16)
        qTa = P.sb("f_qTa", [128, DH, OWN], BF16)
        self.ropr = Ring(P, "ropr", [128, 512], F32, 4)
        rows = P.sb("f_rows", [3, 128], F32)
        gfm = P.sb("f_gfm", [128, 1, 3], F32)
        lrow = P.sb("f_lrow", [1, 4, 64], F32)
        lsc = P.sb("f_lsc", [1, 8], F32)
        nlam = P.sb("f_nlam", [128, 2], F32)
        sg = P.sb("f_sg", [128, 1], F32)
        sqb = Ring(P, "f_sqb", [128, 512], BF16, 3)
        f1 = Ring(P, "f_f", [128, 512], F32, 8)
        for hf in range(2):
            P.dma(rows[0:1, hf * 64:(hf + 1) * 64], io['dqn_g'][:, :], w=['f_rows'])
            P.dma(rows[1:2, hf * 64:(hf + 1) * 64], io['dkn_g'][:, :], w=['f_rows'])
        P.dma(rows[2:3, :], io['subln_g'][:, :], w=['f_rows'])
        P.dma(lrow[0:1, :, :], io['lqk'].rearrange("(o a) k -> o a k", o=1), w=['f_lrow'])
        self.rows_to_fm(rows, 3, 128, gfm[:], 'f_rows', 'f_gfm')
        P.ts(sg[:], gfm[:, 0, 2:3], 1.0 - lam_init, ALU.mult, r=['f_gfm'], w=['f_sg'])
        for i in range(2):
            P.tt(lrow[0:1, 2 * i, :], lrow[0:1, 2 * i, :], lrow[0:1, 2 * i + 1, :], ALU.mult, r=['f_lrow'], w=['f_lrow'])
            P.op('dve', lambda e, o=lsc[0:1, i:i + 1], a=lrow[0:1, 2 * i, :]: e.reduce_sum(out=o, in_=a, axis=AX.X),
                 r=['f_lrow'], w=['f_lsc'])
        P.act(lsc[0:1, 0:2], lsc[0:1, 0:2], AF.Exp, r=['f_lsc'], w=['f_lsc'])
        for i in range(2):
            P.tt(lsc[0:1, 2 + i:3 + i], lsc[0:1, 1:2], lsc[0:1, 0:1], ALU.subtract, r=['f_lsc'], w=['f_lsc'])
        P.ts(lsc[0:1, 2:4], lsc[0:1, 2:4], -lam_init, ALU.add, r=['f_lsc'], w=['f_lsc'])
        P.mm(pb[0][:, 0:2], self.ones32[0:1, 0:128], lsc[0:1, 2:4], r=['ones32', 'f_lsc'], w=['pb0'])
        P.copy(nlam[:], pb[0][:, 0:2], r=['pb0'], w=['f_nlam'])
        qng, kng = gfm[:, 0, 0:1], gfm[:, 0, 1:2]

        def qk_side(w, lhs, rhs, rkeys, gain, dst, dkey, rope):
            for c in range(8):
                P.mm(pb[2][:, 0:w], lhs(c), rhs(c), start=(c == 0), stop=(c == 7), r=rkeys, w=['pb2'])
            s3, s3k = sqb.next()
            P.act(s3[:, 0:w], pb[2][:, 0:w], AF.Square, r=['pb2'], w=[s3k])
            P.mm(pb[3][:, 0:w], self.bd64[:], s3[:, 0:w], r=['bd64', s3k], w=['pb3'])
            rs, rsk = f1.next()
            P.rsqrt(rs[:, 0:w], pb[3][:, 0:w], 1.0 / DHD, r=['pb3'], w=[rsk])
            if rope is None:
                P.stt(dst, pb[2][:, 0:w], gain, rs[:, 0:w], ALU.mult, ALU.mult, r=['pb2', rsk, 'f_gfm'], w=[dkey])
            else:
                kn_, knk = sqb.next()
                P.stt(kn_[:, 0:w], pb[2][:, 0:w], gain, rs[:, 0:w], ALU.mult, ALU.mult, r=['pb2', rsk, 'f_gfm'], w=[knk])
                self.rope_fm(dst, kn_[:, 0:w], 128, w, rope[0], f1, knk, dkey, 1, tabs=rope[1])

        P.push()
        rings = self.norm_rings()
        for gi, (t0, w) in enumerate(self.gA):
            self.norm_tiles_to_hT(self.kvrow, t0, w, hT[:, :, t0:t0 + w], ('hT', gi), 0, rings)
        wq = P.sb("f_wq", [128, 8, 1024], BF16)
        self.load_w(wq, 'f_wq', io['w_in'][:, 0:1024], 8, 1024)
        hqr = Ring(P, "f_hq", [128, 8, 512], BF16, 2)
        for gi, (t0, w) in enumerate(qgroups):
            hq, hqk = hqr.next()
            self.norm_tiles_to_hT(lambda p: io['xl0'][p:p + 128, :], t0, w, hq, hqk, 0, rings, isctx=0)
            for h in range(DH):
                qk_side(w, lambda c: wq[:, c, h * 128:(h + 1) * 128], lambda c: hq[:, c, 0:w], ['f_wq', hqk], qng,
                        qTa[:, h, t0:t0 + w], ('qTa', h, gi), (t0, ('rcos', 'rsin')))
        P.pop()
        whr = Ring(P, "f_wh", [128, 8, 256], BF16, 2)
        kT = P.sb("f_kT", [128, NT], BF16)
        vh = P.sb("f_vh", [128, NT // 128, 128], BF16)
        pT = Ring(P, "f_pT", [128, 512], BF16, 4)
        aor = Ring(P, "f_ao", [128, 512], BF16, 2)
        SC = float(DHD ** -0.5)
        for h in range(DH):
            wh, whk = whr.next()
            for c in range(8):
                for part in range(2):
                    st, sk = self.staged.next()
                    col0 = (part + 1) * 1024 + h * 128
                    P.dma(st[:, 0:128], io['w_in'][c * 128:(c + 1) * 128, col0:col0 + 128], w=[sk])
                    P.copy(wh[:, c, part * 128:(part + 1) * 128], st[:, 0:128], r=[sk], w=[whk], eng='pool')
            P.barrier()
            for gi, (t0, w) in enumerate(self.gA):
                qk_side(w, lambda c: wh[:, c, 0:128], lambda c: hT[:, c, t0:t0 + w], [whk], kng,
                        kT[:, t0:t0 + w], ('kT', gi), None if t0 < C else (t0 - C, ('rcosk', 'rsink')))
                for ti in range(w // 128):
                    a0 = t0 + ti * 128
                    bi = 4 + (ti % 2)
                    for c in range(8):
                        P.mm(pb[bi][:, 0:128], hT[:, c, a0:a0 + 128], wh[:, c, 128:256], start=(c == 0), stop=(c == 7),
                             r=[whk], w=[f'pb{bi}'])
                    P.copy(vh[:, a0 // 128, :], pb[bi][:, 0:128], r=[f'pb{bi}'], w=[('vh', a0)], eng='act')
            P.barrier()
            nkt = NT // 128
            for qi, (q0, w) in enumerate(qgroups):
                for kt_ in range(nkt):
                    k0 = kt_ * 128
                    b1, b2 = kt_ % 2, 2 + kt_ % 2
                    P.mm(pb[b1][:, 0:w], kT[0:64, k0:k0 + 128], qTa[0:64, h, q0:q0 + w], w=[f'pb{b1}'])
                    P.mm(pb[b2][:, 0:w], kT[64:128, k0:k0 + 128], qTa[64:128, h, q0:q0 + w], w=[f'pb{b2}'])
                    p1, p1k = pT.next()
                    p2, p2k = pT.next()
                    P.act(p1[:, 0:w], pb[b1][:, 0:w], AF.Exp, scale=SC, r=[f'pb{b1}'], w=[p1k])
                    P.act(p2[:, 0:w], pb[b2][:, 0:w], AF.Exp, scale=SC, r=[f'pb{b2}'], w=[p2k])
                    st_, sp_ = (kt_ == 0), (kt_ == nkt - 1)
                    P.mm(pb[4][:, 0:w], vh[:, kt_, :], p1[:, 0:w], start=st_, stop=sp_, r=[p1k], w=['pb4'])
                    P.mm(pb[5][:, 0:w], self.onesb[:], p1[:, 0:w], start=st_, stop=sp_, r=[p1k], w=['pb5'])
                    P.mm(pb[6][:, 0:w], vh[:, kt_, :], p2[:, 0:w], start=st_, stop=sp_, r=[p2k], w=['pb6'])
                    P.mm(pb[7][:, 0:w], self.onesb[:], p2[:, 0:w], start=st_, stop=sp_, r=[p2k], w=['pb7'])
                r1, r1k = f1.next()
                r2, r2k = f1.next()
                P.recip(r1[:, 0:w], pb[5][:, 0:w], r=['pb5'], w=[r1k])
                P.recip(r2[:, 0:w], pb[7][:, 0:w], r=['pb7'], w=[r2k])
                P.tt(r1[:, 0:w], pb[4][:, 0:w], r1[:, 0:w], ALU.mult, r=['pb4', r1k], w=[r1k])
                P.tt(r2[:, 0:w], pb[6][:, 0:w], r2[:, 0:w], ALU.mult, r=['pb6', r2k], w=[r2k])
                dd, ddk = f1.next()
                P.stt(dd[:, 0:w], r2[:, 0:w], nlam[:, 0:1], r1[:, 0:w], ALU.mult, ALU.add, r=[r1k, r2k, 'f_nlam'], w=[ddk])
                s3, s3k = sqb.next()
                P.act(s3[:, 0:w], dd[:, 0:w], AF.Square, r=[ddk], w=[s3k])
                P.mm(pb[0][:, 0:w], self.onesb[:], s3[:, 0:w], r=['onesb', s3k], w=['pb0'])
                rs, rsk = f1.next()
                P.rsqrt(rs[:, 0:w], pb[0][:, 0:w], 1.0 / 128, r=['pb0'], w=[rsk])
                ao, aok = aor.next()
                P.stt(ao[:, 0:w], dd[:, 0:w], sg[:, 0:1], rs[:, 0:w], ALU.mult, ALU.mult, r=[ddk, rsk, 'f_sg'], w=[aok])
                P.dma(io['mix'][h, :, q0:q0 + w], ao[:, 0:w], r=[aok])
            P.barrier()
        P.pop()


def build_fused(cfg, ncores):
    sh = LayerBuilder(cfg, 0)
    S, C, NE, NT, OWN = sh.S, sh.C, sh.NE, sh.NT, sh.OWN
    P = sh.P
    for nm, shp in [('ident', [128, 128]), ('prot', [128, 128]), ('rcos', [128, S]), ('rsin', [128, S]),
                    ('cc', [2, D]), ('xa', [NT, D])]:
        sh.din(nm, shp, glob=True)
    sh.gio['rcosk'], sh.gio['rsink'] = sh.io['rcos'], sh.io['rsin']
    sh.io['rcosk'], sh.io['rsink'] = sh.io['rcos'], sh.io['rsin']
    sh.dscr('xl0', [S, D], glob=True)
    sh.dscr('xc0', [C, D], glob=True)
    sh.dscr('mix', [8, 128, NT], BF16, glob=True)
    sh.dscr('h2s', [8, 128, NT], BF16, glob=True)
    sh.dscr('qkvs', [6, 128, NT], BF16, glob=True)
    sh.dscr('x1s', [NT, D], glob=True)
    sh.dscr('gates', [NE, NT], glob=True)
    out_l = sh.dout('out_l', [OWN, D])
    common = [('mod_w', [D, 6 * D]), ('mod_b', [1, 6 * D]), ('n1g', [1, D]), ('n2g', [1, D]),
              ('router_w', [D, NE]), ('router_b', [1, NE]), ('w_gu', [NE, D, 2 * FF]), ('b_gu', [NE, 2 * FF]),
              ('w_down', [NE, FF, D]), ('b_down', [NE, D]), ('w_out', [D, D])]
    L0 = sh
    for nm, shp in common + [('w_in', [D, EVEN_IN]), ('conv5', [5, 512]), ('conv_b', [1, 512]), ('lru_wa', [2, 8, 64, 64]),
                             ('lru_wx', [2, 8, 64, 64]), ('lru_ba', [2, 512]), ('lru_bx', [2, 512]), ('lru_lam', [2, 512]),
                             ('q_norm_g', [1, QR]), ('w_uq', [QR, 768]), ('kv_norm_g', [1, KVR]), ('w_ukv', [KVR, 1024]),
                             ('qn_g', [1, 192]), ('kn_g', [1, 192])]:
        L0.din(nm, shp)
    L0.setup_consts()
    P.push()
    L0.xrow = lambda a0: L0.io['xa'][a0:a0 + 128, :]
    L0.phase_mod()
    L0.phase_in_even()
    L0.phase_lru()
    L0.phase_mla()
    L0.phase_out('w_out')
    L0.phase_moe([(L0.io['xc0'], 0, C), (L0.io['xl0'], C, S)])
    P.pop()
    L1 = LayerBuilder(cfg, 1, shared=sh)
    for nm, shp in common + [('w_in', [D, 3072]), ('dqn_g', [1, 64]), ('dkn_g', [1, 64]), ('lqk', [4, 64]), ('subln_g', [1, 128])]:
        L1.din(nm, shp)
    P.push()
    L1.xrow = lambda a0: L1.io['xl0'][a0 - C:a0 - C + 128, :]
    L1.kvrow = lambda a0: (L1.io['xc0'][a0:a0 + 128, :] if a0 < C else L1.io['xl0'][a0 - C:a0 - C + 128, :])
    L1.phase_mod()
    L1.phase_diff()
    L1.phase_out('w_out')
    L1.phase_moe([(out_l, 0, OWN)])
    P.pop()
    P.finish()
    return sh


def rope_tables_np(S, GW=64, rot_dim=64, base=10000.0):
    n_rows = S // GW
    rows = np.repeat(np.arange(n_rows, dtype=np.float32), GW)
    cols = np.tile(np.arange(GW, dtype=np.float32), n_rows)
    axis_dim = rot_dim // 2
    inv_freq = (base ** (-np.arange(0, axis_dim, 2, dtype=np.float32) / axis_dim)).astype(np.float32)
    ang_r = rows[:, None] * inv_freq
    ang_c = cols[:, None] * inv_freq
    ang = np.concatenate([ang_r, ang_r, ang_c, ang_c], axis=-1)
    return np.cos(ang).astype(np.float32), np.sin(ang).astype(np.float32)


def prot_matrix():
    Pm = np.zeros((128, 128), np.float32)
    for blk in range(2):
        o = blk * 64
        for a in range(2):
            for q in range(16):
                Pm[o + a * 32 + 16 + q, o + a * 32 + q] = -1.0
                Pm[o + a * 32 + q, o + a * 32 + 16 + q] = 1.0
    return Pm


def core_inputs(inp, b, half, cfg, core=None, ncores=8):
    S, C = cfg['S'], cfg['C']
    core = 2 * b + half if core is None else core
    EPC = cfg['NE'] // ncores
    OWN = S // 2
    rev = (half == 1)
    f = (lambda a: a[::-1]) if rev else (lambda a: a)
    A = np.ascontiguousarray
    cos, sin = rope_tables_np(S)
    st2 = lambda t: A(np.concatenate([t.T, t.T], 0))
    pair_order = np.concatenate([np.arange(OWN), np.arange(S - 1, S - 1 - OWN, -1)])
    m = {
        'xa': A(np.concatenate([f(inp['ctx'][b]), f(inp['x'][b])], 0)),
        'cc': A(np.stack([inp['c'][b], inp['c_ctx']], 0)),
        'ident': np.eye(128, dtype=np.float32), 'prot': prot_matrix(),
        'rcos': st2(f(cos)), 'rsin': st2(f(sin)),
    }
    for layer in range(cfg['DEPTH']):
        i = layer // 2
        p = f"L{layer}_"
        m.update({
            p + 'mod_w': A(inp['mod_w'][layer]), p + 'mod_b': A(inp['mod_b'][layer][None]),
            p + 'n1g': A(inp['norm1_g'][layer][None]), p + 'n2g': A(inp['norm2_g'][layer][None]),
            p + 'router_w': A(inp['router_w'][layer]), p + 'router_b': A(inp['router_b'][layer][None]),
            p + 'w_gu': A(inp['moe_w_gu'][layer]), p + 'b_gu': A(inp['moe_b_gu'][layer]),
            p + 'w_down': A(inp['moe_w_down'][layer]), p + 'b_down': A(inp['moe_b_down'][layer]),
        })
        if layer % 2 == 0:
            cw = inp['lru_conv_w'][i]
            z = np.zeros((1, 512), np.float32)
            conv5 = np.concatenate([cw, z], 0) if not rev else np.concatenate([z, cw[::-1]], 0)
            m.update({
                p + 'w_in': A(inp['ev_w_in'][i]), p + 'w_out': A(inp['ev_w_out'][i]), p + 'conv5': A(conv5),
                p + 'conv_b': A(inp['lru_conv_b'][i][None]), p + 'lru_wa': A(f(inp['lru_wa'][i])),
                p + 'lru_wx': A(f(inp['lru_wx'][i])), p + 'lru_ba': A(f(inp['lru_ba'][i])),
                p + 'lru_bx': A(f(inp['lru_bx'][i])), p + 'lru_lam': A(f(inp['lru_lambda'][i])),
                p + 'q_norm_g': A(inp['mla_q_norm_g'][i][None]), p + 'w_uq': A(inp['mla_w_uq'][i]),
                p + 'kv_norm_g': A(inp['mla_kv_norm_g'][i][None]), p + 'w_ukv': A(inp['mla_w_ukv'][i]),
                p + 'qn_g': A(inp['mla_qn_g'][i][None]), p + 'kn_g': A(inp['mla_kn_g'][i][None]),
            })
        else:
            m.update({
                p + 'w_in': A(inp['od_w_in'][i]), p + 'w_out': A(inp['od_w_out'][i]),
                p + 'dqn_g': A(inp['diff_qn_g'][i][None]), p + 'dkn_g': A(inp['diff_kn_g'][i][None]),
                p + 'lqk': A(np.stack([inp['diff_lq1'][i], inp['diff_lk1'][i], inp['diff_lq2'][i], inp['diff_lk2'][i]], 0)),
                p + 'subln_g': A(inp['diff_subln_g'][i][None]),
            })
    return m


def gather_outputs(results, cfg, nb):
    S = cfg['S']
    OWN = S // 2
    xl = np.zeros((nb, S, D), np.float32)
    for core, r in enumerate(results):
        b, half = core // 2, core % 2
        if half == 0:
            xl[b, 0:OWN] = r['out_l']
        else:
            xl[b, S - OWN:S] = r['out_l'][::-1]
    return xl


CFG = dict(S=4096, C=256, NE=32, DEPTH=2)
_PROG = {}


def kernel(**inputs):
    inp = {k: np.asarray(v) for k, v in inputs.items()}
    nb = inp['x'].shape[0]
    ncore = 2 * nb
    if ncore not in _PROG:
        _PROG[ncore] = build_fused(CFG, ncore)
    B = _PROG[ncore]
    in_maps = [core_inputs(inp, c // 2, c % 2, CFG, core=c, ncores=ncore) for c in range(ncore)]
    res = run_bass_kernel_spmd(B.nc, in_maps, core_ids=list(range(ncore)))
    return gather_outputs(res.results, CFG, nb)
```

```python
import numpy as np
from contextlib import ExitStack
import concourse.bass as bass
import concourse.mybir as mybir
from concourse.bass_utils import run_bass_kernel_spmd

F32 = mybir.dt.float32
BF16 = mybir.dt.bfloat16
AF = mybir.ActivationFunctionType
ALU = mybir.AluOpType
AX = mybir.AxisListType

EPS = 1e-6
D = 1024
DC = 8
LRU_W = 512
QR, KVR, ROPE, NOPE, VD, MH = 384, 256, 64, 128, 128, 4
EVEN_IN = 1728
DH, DHD = 8, 64
FF = 1024
TOPK = 4
LIMIT = 7.0
ALPHA = 1.702


class Prog:
    ENGS = ('pe', 'dve', 'act', 'pool', 'sp')

    def __init__(self, nc, ndma=16):
        self.nc = nc
        self.h = {'pe': nc.tensor, 'dve': nc.vector, 'act': nc.scalar, 'pool': nc.gpsimd, 'sp': nc.sync}
        self.sem = {e: nc.alloc_semaphore(name=f"s_{e}") for e in self.ENGS}
        self.dsem = [nc.alloc_semaphore(name=f"s_d{i}") for i in range(ndma)]
        self.cnt = {e: 0 for e in self.ENGS}
        self.dcnt = [0] * ndma
        self.dnext = 0
        self.known = {e: {} for e in self.ENGS}
        self.lastw = {}
        self.readers = {}
        self.ops = {e: [] for e in self.ENGS}
        self.stack = [ExitStack()]
        self.n_inst = 0
        self.csem = []

    def sb(self, name, shape, dtype):
        self.n_alloc = getattr(self, 'n_alloc', 0) + 1
        return self.stack[-1].enter_context(self.nc.sbuf_tensor(f"sb{self.n_alloc}_{name}", list(shape), dtype))

    def ps(self, name, shape, dtype):
        return self.stack[-1].enter_context(self.nc.psum_tensor(f"ps_{name}", list(shape), dtype))

    def push(self):
        self.stack.append(ExitStack())

    def pop(self):
        self.barrier()
        self.stack.pop().close()

    def _deps(self, eng, reads, writes):
        deps = {}
        def add(t):
            if t is None:
                return
            k, v = t
            if deps.get(k, 0) < v:
                deps[k] = v
        for k in reads:
            add(self.lastw.get(k))
        for k in writes:
            add(self.lastw.get(k))
            for t in self.readers.get(k, ()):
                add(t)
        waits = []
        for k, v in deps.items():
            if k == 'pe' and eng == 'pe':
                continue
            if self.known[eng].get(k, 0) >= v:
                continue
            self.known[eng][k] = v
            waits.append((k, v))
        return waits

    def _commit(self, tok, reads, writes):
        for k in writes:
            self.lastw[k] = tok
            self.readers[k] = []
        for k in reads:
            self.readers.setdefault(k, []).append(tok)

    def op(self, eng, fn, r=(), w=()):
        waits = self._deps(eng, r, w)
        self.cnt[eng] += 1
        tok = (eng, self.cnt[eng])
        self.ops[eng].append((waits, fn, ('c', eng)))
        self._commit(tok, r, w)
        self.n_inst += 1
        return tok

    def dma(self, out, in_, r=(), w=(), q='sp', **kw):
        nsp = len(self.dsem) - 4
        if q == 'sp':
            s = self.dnext
            self.dnext = (self.dnext + 1) % nsp
        else:
            self.dnext2 = (getattr(self, 'dnext2', -1) + 1) % 4
            s = nsp + self.dnext2
        waits = self._deps(q, r, w)
        key = ('d', s)
        prev = 16 * self.dcnt[s]
        if prev > 0 and self.known[q].get(key, 0) < prev:
            self.known[q][key] = prev
            waits.append((key, prev))
        self.dcnt[s] += 1
        tok = (key, 16 * self.dcnt[s])
        self.ops[q].append((waits, lambda e: e.dma_start(out=out, in_=in_, **kw), ('d', s)))
        self._commit(tok, r, w)
        self.n_inst += 1
        return tok

    def coll_async(self, fn, deps=()):
        h = self.nc.alloc_semaphore(name=f"s_c{len(self.csem)}")
        self.csem.append(h)
        idx = len(self.csem) - 1
        waits = []
        for (k, v) in deps:
            if self.known['pool'].get(k, 0) < v:
                self.known['pool'][k] = v
                waits.append((k, v))
        self.ops['pool'].append((waits, fn, ('x', idx)))
        self.n_inst += 1
        return (('x', idx), 16)

    def wait_tok(self, eng, tok):
        k, v = tok
        if self.known[eng].get(k, 0) < v:
            self.known[eng][k] = v
            self.ops[eng].append(([(k, v)], None, None))

    def coll(self, fn):
        self.barrier()
        s = self.dnext
        self.dnext = (self.dnext + 1) % (len(self.dsem) - 4)
        self.dcnt[s] += 1
        self.ops['pool'].append(([], fn, ('d', s)))
        self.n_inst += 1
        self.barrier()

    def barrier(self):
        for e in self.ENGS:
            waits = []
            for e2 in self.ENGS:
                if e2 != e and self.cnt[e2] > self.known[e].get(e2, 0):
                    waits.append((e2, self.cnt[e2]))
                    self.known[e][e2] = self.cnt[e2]
            for s in range(len(self.dsem)):
                v = 16 * self.dcnt[s]
                if v > self.known[e].get(('d', s), 0):
                    waits.append((('d', s), v))
                    self.known[e][('d', s)] = v
            if waits:
                self.ops[e].append((waits, None, None))
        self.lastw = {}
        self.readers = {}

    def _semh(self, k):
        if isinstance(k, str):
            return self.sem[k]
        return self.dsem[k[1]] if k[0] == 'd' else self.csem[k[1]]

    def finish(self):
        for i in range(len(self.csem)):
            self.wait_tok('sp', (('x', i), 16))
        self.barrier()
        with self.nc.Block() as block:
            def replay(ename, e):
                for waits, fn, inc in self.ops[ename]:
                    for k, v in waits:
                        e.wait_ge(self._semh(k), v)
                    if fn is None:
                        continue
                    ins = fn(e)
                    if inc[0] == 'c':
                        ins.then_inc(self.sem[inc[1]], 1)
                    elif inc[0] == 'd':
                        ins.then_inc(self.dsem[inc[1]], 16)
                    else:
                        ins.then_inc(self.csem[inc[1]], 16)

            @block.tensor
            def _(e):
                replay('pe', e)

            @block.vector
            def _(e):
                replay('dve', e)

            @block.scalar
            def _(e):
                replay('act', e)

            @block.gpsimd
            def _(e):
                replay('pool', e)

            @block.sync
            def _(e):
                replay('sp', e)
        while self.stack:
            self.stack.pop().close()

    def mm(self, out, lhsT, rhs, start=True, stop=True, r=(), w=()):
        return self.op('pe', lambda e: e.matmul(out, lhsT, rhs, start=start, stop=stop), r, w)

    def tr(self, out, in_, ident, r=(), w=()):
        return self.op('pe', lambda e: e.transpose(out, in_, ident), r, w)

    def act(self, out, in_, func, bias=None, scale=None, accum=None, r=(), w=()):
        kw = {}
        if bias is not None:
            kw['bias'] = bias
        if scale is not None:
            kw['scale'] = scale
        if accum is not None:
            kw['accum_out'] = accum
        return self.op('act', lambda e: e.activation(out=out, in_=in_, func=func, **kw), r, w)

    def tt(self, out, in0, in1, op, r=(), w=(), eng='dve'):
        return self.op(eng, lambda e: e.tensor_tensor(out=out, in0=in0, in1=in1, op=op), r, w)

    def ts(self, out, in0, s1, op0, s2=None, op1=None, r=(), w=(), eng='dve'):
        if op1 is None:
            return self.op(eng, lambda e: e.tensor_scalar(out=out, in0=in0, scalar1=s1, scalar2=None, op0=op0), r, w)
        return self.op(eng, lambda e: e.tensor_scalar(out=out, in0=in0, scalar1=s1, scalar2=s2, op0=op0, op1=op1), r, w)

    def stt(self, out, in0, scalar, in1, op0, op1, r=(), w=(), eng='dve'):
        return self.op(eng, lambda e: e.scalar_tensor_tensor(out=out, in0=in0, scalar=scalar, in1=in1, op0=op0, op1=op1), r, w)

    def recip(self, out, in_, r=(), w=()):
        return self.op('dve', lambda e: e.reciprocal(out=out, in_=in_), r, w)

    def copy(self, out, in_, r=(), w=(), eng='dve'):
        if eng == 'act':
            return self.op('act', lambda e: e.copy(out=out, in_=in_), r, w)
        return self.op(eng, lambda e: e.tensor_copy(out=out, in_=in_), r, w)

    def memset(self, ap, val, w=(), eng='dve'):
        return self.op(eng, lambda e: e.memset(ap, val), (), w)

    def scan(self, out, d0, d1, initial, r=(), w=()):
        return self.op('dve', lambda e: e.tensor_tensor_scan(out=out, data0=d0, data1=d1, initial=initial,
                                                              op0=ALU.mult, op1=ALU.add), r, w)

    def rsqrt(self, out, in_, scale, r=(), w=()):
        np_ = out.partition_size()
        self.act(out, in_, AF.Sqrt, bias=self.epsc[0:np_, 0:1], scale=scale, r=list(r) + ['epsc'], w=w)
        return self.recip(out, out, r=w, w=w)


def groups(s, e, gw=512):
    return [(t, min(gw, e - t)) for t in range(s, e, gw)]


class Ring:
    def __init__(self, P, name, shape, dtype, n=2):
        self.t = [P.sb(f"{name}{i}", shape, dtype) for i in range(n)]
        self.k = [f"{name}{i}" for i in range(n)]
        self.i = -1

    def next(self):
        self.i = (self.i + 1) % len(self.t)
        return self.t[self.i], self.k[self.i]


class LayerBuilder:
    def __init__(self, cfg, layer, shared=None):
        self.cfg = cfg
        self.layer = layer
        S, C, NE = cfg['S'], cfg['C'], cfg['NE']
        self.S, self.C, self.NE = S, C, NE
        self.NT = C + S
        self.OWN = S // 2
        self.even = (layer % 2 == 0)
        self.last = (layer == cfg['DEPTH'] - 1)
        self.Q0 = C if self.last else 0
        self.Q1 = (C + self.OWN) if self.last else self.NT
        self.TQ = self.Q1 - self.Q0
        gw = min(512, self.OWN)
        self.gA = groups(0, C, gw) + groups(C, C + self.OWN, gw) + groups(C + self.OWN, self.NT, gw)
        self.gQ = [g for g in self.gA if self.Q0 <= g[0] < self.Q1]
        self.prefix = f"L{layer}_"
        if shared is None:
            self.nc = bass.Bass("TRN2", target_bir_lowering=False)
            self.P = Prog(self.nc)
            self.io = {}
        else:
            self.nc, self.P = shared.nc, shared.P
            self.io = dict(shared.gio)
            for k in ('ident', 'identb', 'ones32', 'onesb', 'bd64', 'prot', 'pb', 'staged'):
                setattr(self, k, getattr(shared, k))
        self.gio = {}

    def din(self, name, shape, dt=F32, glob=False):
        nm = name if glob else self.prefix + name
        self.io[name] = self.nc.dram_tensor(nm, list(shape), dt, kind="ExternalInput").ap()
        if glob:
            self.gio[name] = self.io[name]
        return self.io[name]

    def dout(self, name, shape, dt=F32):
        self.io[name] = self.nc.dram_tensor(name, list(shape), dt, kind="ExternalOutput").ap()
        return self.io[name]

    def dscr(self, name, shape, dt=F32, glob=False):
        nm = name if glob else self.prefix + name
        self.io[name] = self.nc.dram_tensor(nm, list(shape), dt, kind="Internal").ap()
        if glob:
            self.gio[name] = self.io[name]
        return self.io[name]

    def setup_consts(self):
        P, io = self.P, self.io
        self.ident = P.sb("ident", [128, 128], F32)
        self.identb = P.sb("identb", [128, 128], BF16)
        self.ones32 = P.sb("ones32", [128, 128], F32)
        self.onesb = P.sb("onesb", [128, 128], BF16)
        self.bd64 = P.sb("bd64", [128, 128], BF16)
        self.protf = P.sb("protf", [128, 128], F32)
        self.prot = P.sb("prot", [128, 128], BF16)
        P.epsc = P.sb("epsc", [128, 1], F32)
        self.pb = [P.ps(f"pb{i}", [128, 512], F32) for i in range(8)]
        P.dma(self.ident[:], io['ident'][:, :], w=['ident'])
        P.dma(self.protf[:], io['prot'][:, :], w=['protf'])
        P.copy(self.identb[:], self.ident[:], r=['ident'], w=['identb'])
        P.copy(self.prot[:], self.protf[:], r=['protf'], w=['prot'])
        P.memset(self.ones32[:], 1.0, w=['ones32'])
        P.memset(self.onesb[:], 1.0, w=['onesb'])
        P.memset(self.bd64[:], 0.0, w=['bd64'])
        P.memset(self.bd64[0:64, 0:64], 1.0, w=['bd64'])
        P.memset(self.bd64[64:128, 64:128], 1.0, w=['bd64'])
        P.memset(P.epsc[:], EPS, w=['epsc'])
        self.staged = Ring(P, "wstg", [128, 1024], F32, 2)
        P.barrier()

    def load_w(self, dst, dkey, src, kc, F, scale_col=None, skey=None):
        P = self.P
        for c in range(kc):
            for f0 in range(0, F, 1024):
                fw = min(1024, F - f0)
                st, sk = self.staged.next()
                P.dma(st[:, 0:fw], src[c * 128:(c + 1) * 128, f0:f0 + fw], w=[sk])
                if scale_col is None:
                    P.copy(dst[:, c, f0:f0 + fw], st[:, 0:fw], r=[sk], w=[dkey], eng='pool')
                else:
                    P.ts(dst[:, c, f0:f0 + fw], st[:, 0:fw], scale_col[:, c:c + 1], ALU.mult, r=[sk, skey], w=[dkey])

    def rows_to_fm(self, rows, R, F, out, rkey, okey):
        P = self.P
        pb = self.pb[7]
        nchunk = F // 128
        for c in range(nchunk):
            P.tr(pb[:, c * R:(c + 1) * R], rows[0:R, c * 128:(c + 1) * 128], self.ident[0:R, 0:R],
                 r=[rkey, 'ident'], w=['pb7'])
        P.copy(out, pb[:, 0:nchunk * R].rearrange("p (c r) -> p c r", r=R), r=['pb7'], w=[okey])

    def phase_mod(self):
        P, io = self.P, self.io
        self.modfm = P.sb("modfm", [128, 48, 2], F32)
        self.Gb = P.sb("Gb", [128, 2, 2, D], F32)
        self.AB = P.sb("AB", [128, 4, 2, 8], F32)
        P.push()
        rows = P.sb("m_rows", [4, D], F32)
        sig = P.sb("m_sig", [2, D], F32)
        fm = P.sb("m_fm", [128, 8, 4], F32)
        sTb = P.sb("m_sTb", [128, 8, 2, 128], F32)
        modb = P.sb("m_modb", [1, 6 * D], F32)
        wr = Ring(P, "m_w", [128, 8, 512], F32, 2)
        P.dma(rows[0:2, :], io['cc'][:, :], w=['m_rows'])
        P.dma(rows[2:3, :], io['n1g'][:, :], w=['m_rows'])
        P.dma(rows[3:4, :], io['n2g'][:, :], w=['m_rows'])
        P.dma(modb[:], io['mod_b'][:, :], w=['m_modb'])
        P.act(sig[:], rows[0:2, :], AF.Sigmoid, r=['m_rows'], w=['m_sig'])
        P.tt(rows[0:2, :], rows[0:2, :], sig[:], ALU.mult, r=['m_rows', 'm_sig'], w=['m_rows'])
        self.rows_to_fm(rows, 4, D, fm[:], 'm_rows', 'm_fm')
        for c in range(8):
            for v in range(2):
                P.act(sTb[:, c, v, :], self.ones32[:], AF.Identity, scale=fm[:, c, v:v + 1],
                      r=['ones32', 'm_fm'], w=['m_sTb'])
        mw = io['mod_w'].rearrange("(c p) j -> p c j", p=128)
        for jb in range(12):
            piece, hf = jb // 2, jb % 2
            wt, wk = wr.next()
            P.dma(wt[:], mw[:, :, jb * 512:(jb + 1) * 512], w=[wk])
            pf = self.pb[0]
            for jj in range(4):
                j = jb * 4 + jj
                for c in range(8):
                    P.mm(pf[:, jj * 2:jj * 2 + 2], wt[:, c, jj * 128:(jj + 1) * 128], fm[:, c, 0:2],
                         start=(c == 0), stop=False, r=[wk, 'm_fm'], w=['pb0'])
                P.mm(pf[:, jj * 2:jj * 2 + 2], modb[0:1, j * 128:(j + 1) * 128], self.ones32[0:1, 0:2],
                     start=False, stop=True, r=['m_modb', 'ones32'], w=['pb0'])
            P.copy(self.modfm[:, jb * 4:(jb + 1) * 4, :], pf[:, 0:8].rearrange("p (j v) -> p j v", v=2),
                   r=['pb0'], w=['modfm'])
            if piece in (2, 5):
                gi = 0 if piece == 2 else 1
                for v in range(2):
                    pg = self.pb[1 + v]
                    for c in range(8):
                        P.mm(pg[:, :], sTb[:, c, v, :], wt[:, c, :], start=(c == 0), stop=False,
                             r=[wk, 'm_sTb'], w=[f'pb{1 + v}'])
                    P.mm(pg[:, :], self.ones32[0:1, 0:128], modb[0:1, jb * 512:(jb + 1) * 512],
                         start=False, stop=True, r=['m_modb', 'ones32'], w=[f'pb{1 + v}'])
                    P.copy(self.Gb[:, gi, v, hf * 512:(hf + 1) * 512], pg[:, :], r=[f'pb{1 + v}'], w=['Gb'], eng='act')
        for v in range(2):
            P.stt(self.AB[:, 0, v, :], self.modfm[:, 8:16, v], 1.0, fm[:, :, 2], ALU.add, ALU.mult,
                  r=['modfm', 'm_fm'], w=['AB'])
            P.copy(self.AB[:, 1, v, :], self.modfm[:, 0:8, v], r=['modfm'], w=['AB'])
            P.stt(self.AB[:, 2, v, :], self.modfm[:, 32:40, v], 1.0, fm[:, :, 3], ALU.add, ALU.mult,
                  r=['modfm', 'm_fm'], w=['AB'])
            P.copy(self.AB[:, 3, v, :], self.modfm[:, 24:32, v], r=['modfm'], w=['AB'])
        P.pop()

    def norm_tiles_to_hT(self, rowfn, t0, w, hT, hk, which, rings, isctx=None):
        P = self.P
        for ti in range(w // 128):
            a0 = t0 + ti * 128
            v = (1 if a0 < self.C else 0) if isctx is None else isctx
            xt, xk = rings['xt'].next()
            P.dma(xt[:], rowfn(a0), w=[xk])
            self.norm_one(xt, xk, v, hT, hk, ti, which, rings)

    def norm_one(self, xt, xk, v, hT, hk, ti, which, rings):
        P = self.P
        jk, jkk = rings['junk'].next()
        st, stk = rings['stat'].next()
        P.act(jk[:], xt[:], AF.Square, accum=st[:, 0:1], r=[xk], w=[jkk, stk])
        P.rsqrt(st[:, 1:2], st[:, 0:1], 1.0 / D, r=[stk], w=[stk])
        xn, xnk = rings['xn'].next()
        P.ts(xn[:], xt[:], st[:, 1:2], ALU.mult, r=[xk, stk], w=[xnk])
        pbi = rings['pbi']
        rings['pbi'] = 1 - pbi
        pt = self.pb[pbi][:].bitcast(BF16)
        for c in range(8):
            P.tr(pt[:, c * 128:(c + 1) * 128], xn[:, c * 128:(c + 1) * 128], self.identb[:],
                 r=[xnk, 'identb'], w=[f'pb{pbi}'])
        for c in range(8):
            P.act(hT[:, c, ti * 128:(ti + 1) * 128], pt[:, c * 128:(c + 1) * 128], AF.Identity,
                  scale=self.AB[:, which, v, c:c + 1], bias=self.AB[:, which + 1, v, c:c + 1],
                  r=[f'pb{pbi}', 'AB'], w=[hk])

    def norm_rings(self):
        P = self.P
        return {'xt': Ring(P, "n_xt", [128, D], F32, 2), 'junk': Ring(P, "n_junk", [128, D], BF16, 1),
                'stat': Ring(P, "n_stat", [128, 2], F32, 2), 'xn': Ring(P, "n_xn", [128, D], BF16, 2), 'pbi': 0}

    def phase_in_even(self):
        P, io = self.P, self.io
        NT, TQ = self.NT, self.Q1
        P.push()
        self.rT = P.sb("rT", [128, 4, NT], BF16)
        self.gT = P.sb("gT", [128, 4, self.Q1], BF16)
        P.push()
        win = P.sb("a_win", [128, 8, EVEN_IN], BF16)
        self.load_w(win, 'a_win', io['w_in'], 8, EVEN_IN)
        rings = self.norm_rings()
        hr = Ring(P, "a_hT", [128, 8, 512], BF16, 2)
        evr = Ring(P, "a_ev", [128, 512], BF16, 3)
        scr_idx = {'qc': 0, 'kvc': 3, 'kr': 5}
        chunks = [('g', j * 128, 128, j) for j in range(4)] + [('r', 512 + j * 128, 128, j) for j in range(4)] + \
                 [('qc', 1024 + j * 128, 128, j) for j in range(3)] + [('kvc', 1408 + j * 128, 128, j) for j in range(2)] + \
                 [('kr', 1664, 64, 0)]
        k = 0
        for gi, (t0, w) in enumerate(self.gA):
            hT, hk = hr.next()
            self.norm_tiles_to_hT(lambda a0: io['xa'][a0:a0 + 128, :], t0, w, hT, hk, 0, rings)
            inq = t0 < self.Q1
            for (nm, c0, cw, j) in chunks:
                if nm in ('g', 'qc') and not inq:
                    continue
                bi = 2 + (k % 4)
                k += 1
                ps = self.pb[bi]
                for c in range(8):
                    P.mm(ps[0:cw, 0:w], win[:, c, c0:c0 + cw], hT[:, c, 0:w], start=(c == 0), stop=(c == 7),
                         r=['a_win', hk], w=[f'pb{bi}'])
                if nm in ('g', 'r'):
                    dst = self.gT if nm == 'g' else self.rT
                    P.copy(dst[:, j, t0:t0 + w], ps[0:cw, 0:w], r=[f'pb{bi}'], w=[(nm, gi)], eng=('act' if k % 2 else 'dve'))
                else:
                    ev, evk = evr.next()
                    P.copy(ev[0:cw, 0:w], ps[0:cw, 0:w], r=[f'pb{bi}'], w=[evk], eng=('act' if k % 2 else 'dve'))
                    P.dma(io['qkvs'][scr_idx[nm] + j, 0:cw, t0:t0 + w], ev[0:cw, 0:w], r=[evk])
        P.pop()

    def phase_lru(self):
        P, io = self.P, self.io
        NT, C, Q1 = self.NT, self.C, self.Q1
        P.push()
        rows = P.sb("b_rows", [12, 512], F32)
        fm = P.sb("b_fm", [128, 4, 12], F32)
        cl = P.sb("b_cl", [128, 4, 2], F32)
        wbd = P.sb("b_wbd", [128, 16, 128], BF16)
        P.push()
        wst = P.sb("b_wst", [128, 16, 128], F32)
        P.dma(rows[0:5, :], io['conv5'][:, :], w=['b_rows'])
        P.dma(rows[5:6, :], io['conv_b'][:, :], w=['b_rows'])
        P.dma(rows[6:8, :], io['lru_ba'][:, :], w=['b_rows'])
        P.dma(rows[8:10, :], io['lru_bx'][:, :], w=['b_rows'])
        P.dma(rows[10:12, :], io['lru_lam'][:, :], w=['b_rows'])
        self.rows_to_fm(rows, 12, 512, fm[:], 'b_rows', 'b_fm')
        P.act(cl[:], fm[:, :, 10:12], AF.Exp, scale=-1.0, r=['b_fm'], w=['b_cl'])
        P.ts(cl[:], cl[:], 1.0, ALU.add, r=['b_cl'], w=['b_cl'])
        P.act(cl[:], cl[:], AF.Ln, r=['b_cl'], w=['b_cl'])
        P.ts(cl[:], cl[:], -8.0, ALU.mult, r=['b_cl'], w=['b_cl'])
        P.memset(wst[:], 0.0, w=['b_wst'])
        for d in range(2):
            for gt, nm in enumerate(('lru_wa', 'lru_wx')):
                for s in range(2):
                    i0 = (d * 2 + gt) * 4
                    srcw = io[nm][d].rearrange("(j s) k m -> s k j m", s=2)[s]
                    P.dma(wst[s * 64:(s + 1) * 64, i0:i0 + 4, s * 64:(s + 1) * 64], srcw, w=['b_wst'])
        P.copy(wbd[:], wst[:], r=['b_wst'], w=['b_wbd'])
        ta = Ring(P, "b_ta", [128, 512], F32, 2)
        tb = Ring(P, "b_tb", [128, 512], F32, 2)
        for gi, (t0, w) in enumerate(groups(0, Q1)):
            for j in range(4):
                g_ap = self.gT[:, j, t0:t0 + w]
                a, ak = ta.next()
                b, bk = tb.next()
                gk = ('gT', j, gi)
                P.tt(a[:, 0:w], g_ap, g_ap, ALU.mult, r=[gk], w=[ak])
                P.ts(a[:, 0:w], a[:, 0:w], 0.044715, ALU.mult, 1.0, ALU.add, r=[ak], w=[ak])
                P.tt(a[:, 0:w], a[:, 0:w], g_ap, ALU.mult, r=[ak, gk], w=[ak])
                P.act(b[:, 0:w], a[:, 0:w], AF.Sigmoid, scale=1.5957691216057308, r=[ak], w=[bk])
                P.tt(g_ap, b[:, 0:w], g_ap, ALU.mult, r=[bk, gk], w=[gk])
        P.pop()
        xcv = P.sb("b_xcv", [128, NT], F32)
        xcvb = P.sb("b_xcvb", [128, NT], BF16)
        recF = P.sb("b_recF", [128, 4, Q1], BF16)
        tr_ = Ring(P, "b_rg", [128, 512], F32, 2)
        ti_ = Ring(P, "b_ig", [128, 512], F32, 2)
        taa = Ring(P, "b_aa", [128, 512], F32, 2)
        tm_ = Ring(P, "b_mm", [128, 512], F32, 2)
        tu_ = Ring(P, "b_uu", [128, 512], F32, 2)
        ths = Ring(P, "b_hs", [128, 512], F32, 2)
        seqs = [(0, C), (C, NT)]
        blocks_f = list(self.gA)
        blocks_b = [g for g in self.gA if g[0] < C][::-1] + [g for g in self.gA if g[0] >= C][::-1]
        kk = 0
        for j in range(4):
            for (s0, s1) in seqs:
                P.ts(xcv[:, s0:s1], self.rT[:, j, s0:s1], fm[:, j, 2:3], ALU.mult, fm[:, j, 5:6], ALU.add,
                     r=['b_fm'], w=['b_xcv'])
                for o in (-2, -1, 1, 2):
                    d0, d1 = max(s0, s0 - o), min(s1, s1 - o)
                    P.stt(xcv[:, d0:d1], self.rT[:, j, d0 + o:d1 + o], fm[:, j, o + 2:o + 3], xcv[:, d0:d1],
                          ALU.mult, ALU.add, r=['b_fm', 'b_xcv'], w=['b_xcv'])
            P.copy(xcvb[:], xcv[:], r=['b_xcv'], w=['b_xcvb'], eng='act')
            for d in range(2):
                prev = None
                for (t0, w) in (blocks_f if d == 0 else blocks_b):
                    kk += 1
                    b0, b1 = kk % 2, 2 + kk % 2
                    P.mm(self.pb[b0][:, 0:w], wbd[:, (d * 2 + 0) * 4 + j, :], xcvb[:, t0:t0 + w],
                         r=['b_wbd', 'b_xcvb'], w=[f'pb{b0}'])
                    P.mm(self.pb[b1][:, 0:w], wbd[:, (d * 2 + 1) * 4 + j, :], xcvb[:, t0:t0 + w],
                         r=['b_wbd', 'b_xcvb'], w=[f'pb{b1}'])
                    rg, rgk = tr_.next()
                    ig, igk = ti_.next()
                    aa, aak = taa.next()
                    mm_, mk = tm_.next()
                    uu, uk = tu_.next()
                    hs, hk = ths.next()
                    P.act(rg[:, 0:w], self.pb[b0][:, 0:w], AF.Sigmoid, bias=fm[:, j, 6 + d:7 + d], r=[f'pb{b0}', 'b_fm'], w=[rgk])
                    P.act(ig[:, 0:w], self.pb[b1][:, 0:w], AF.Sigmoid, bias=fm[:, j, 8 + d:9 + d], r=[f'pb{b1}', 'b_fm'], w=[igk])
                    P.act(aa[:, 0:w], rg[:, 0:w], AF.Exp, scale=cl[:, j, d:d + 1], r=[rgk, 'b_cl'], w=[aak])
                    P.tt(mm_[:, 0:w], aa[:, 0:w], aa[:, 0:w], ALU.mult, r=[aak], w=[mk], eng='pool')
                    P.ts(mm_[:, 0:w], mm_[:, 0:w], -1.0, ALU.mult, 1.0, ALU.add, r=[mk], w=[mk], eng='pool')
                    P.act(mm_[:, 0:w], mm_[:, 0:w], AF.Sqrt, r=[mk], w=[mk])
                    P.tt(uu[:, 0:w], mm_[:, 0:w], ig[:, 0:w], ALU.mult, r=[mk, igk], w=[uk], eng='pool')
                    P.tt(uu[:, 0:w], uu[:, 0:w], xcv[:, t0:t0 + w], ALU.mult, r=[uk, 'b_xcv'], w=[uk], eng='pool')
                    init = 0.0 if prev is None else prev[0][:, prev[2] - 1:prev[2]]
                    rk = [aak, uk] + ([] if prev is None else [prev[1]])
                    if d == 0:
                        P.scan(hs[:, 0:w], aa[:, 0:w], uu[:, 0:w], init, r=rk, w=[hk])
                    else:
                        P.scan(hs[:, 0:w], aa[:, 0:w][:, ::-1], uu[:, 0:w][:, ::-1], init, r=rk, w=[hk])
                    prev = (hs, hk, w)
                    if t0 < Q1:
                        if d == 0:
                            P.copy(recF[:, j, t0:t0 + w], hs[:, 0:w], r=[hk], w=[('recF', j, t0)], eng='act')
                        else:
                            P.tt(mm_[:, 0:w], hs[:, 0:w][:, ::-1], recF[:, j, t0:t0 + w], ALU.add,
                                 r=[hk, ('recF', j, t0)], w=[mk])
                            P.tt(self.gT[:, j, t0:t0 + w], mm_[:, 0:w], self.gT[:, j, t0:t0 + w], ALU.mult,
                                 r=[mk], w=[('gTo', j, t0)])
        P.barrier()
        for j in range(4):
            P.dma(io['mix'][j, :, 0:Q1], self.gT[:, j, :])
        P.pop()
        P.pop()
        P.push()
        self.qcT = P.sb("qcT", [128, 3, self.Q1], BF16)
        self.kvcT = P.sb("kvcT", [128, 2, NT], BF16)
        self.krT = P.sb("krT", [64, NT], BF16)
        for j in range(3):
            P.dma(self.qcT[:, j, :], io['qkvs'][j, :, 0:self.Q1])
        for j in range(2):
            P.dma(self.kvcT[:, j, :], io['qkvs'][3 + j, :, :])
        P.dma(self.krT[:, :], io['qkvs'][5, 0:64, :])
        P.barrier()

    def rope_fm(self, dst, src_bf, np_, w, pos0, tmpr, skey, dkey, pbi, tabs=('rcos', 'rsin')):
        P = self.P
        ps = self.pb[pbi]
        P.mm(ps[0:np_, 0:w], self.prot[0:np_, 0:np_], src_bf, r=['prot', skey], w=[f'pb{pbi}'])
        t1, t1k = tmpr.next()
        t2, t2k = tmpr.next()
        cs, csk = self.ropr.next()
        sn, snk = self.ropr.next()
        P.dma(cs[0:np_, 0:w], self.io[tabs[0]][0:np_, pos0:pos0 + w], w=[csk])
        P.dma(sn[0:np_, 0:w], self.io[tabs[1]][0:np_, pos0:pos0 + w], w=[snk])
        P.tt(t1[0:np_, 0:w], src_bf, cs[0:np_, 0:w], ALU.mult, r=[skey, csk], w=[t1k], eng='pool')
        P.tt(t2[0:np_, 0:w], ps[0:np_, 0:w], sn[0:np_, 0:w], ALU.mult, r=[f'pb{pbi}', snk], w=[t2k])
        P.tt(dst, t1[0:np_, 0:w], t2[0:np_, 0:w], ALU.add, r=[t1k, t2k], w=[dkey])

    def load_rope(self):
        P, io = self.P, self.io
        self.rcos = P.sb("rcos", [128, self.S], F32)
        self.rsin = P.sb("rsin", [128, self.S], F32)
        P.dma(self.rcos[:], io['rcos'][:, :], w=['rope'])
        P.dma(self.rsin[:], io['rsin'][:, :], w=['rope'])

    def phase_mla(self):
        P, io = self.P, self.io
        NT, C, Q1 = self.NT, self.C, self.Q1
        pb = self.pb
        P.push()
        self.ropr = Ring(P, "ropr", [128, 512], F32, 4)
        aor = Ring(P, "c_ao", [128, 512], BF16, 2)
        rows = P.sb("c_rows", [4, 384], F32)
        fm = P.sb("c_fm", [128, 3, 4], F32)
        P.memset(rows[:], 0.0, w=['c_rows'])
        P.dma(rows[0:1, 0:384], io['q_norm_g'][:, :], w=['c_rows'])
        P.dma(rows[1:2, 0:256], io['kv_norm_g'][:, :], w=['c_rows'])
        P.dma(rows[2:3, 0:192], io['qn_g'][:, :], w=['c_rows'])
        P.dma(rows[3:4, 0:192], io['kn_g'][:, :], w=['c_rows'])
        self.rows_to_fm(rows, 4, 384, fm[:], 'c_rows', 'c_fm')
        gq = P.sb("c_gq", [128, 3], F32)
        gkv = P.sb("c_gkv", [128, 2], F32)
        P.copy(gq[:], fm[:, :, 0], r=['c_fm'], w=['c_gq'])
        P.copy(gkv[:], fm[:, 0:2, 1], r=['c_fm'], w=['c_gkv'])
        wq = P.sb("c_wq", [128, 3, 768], BF16)
        wkv = P.sb("c_wkv", [128, 2, 1024], BF16)
        self.load_w(wq, 'c_wq', io['w_uq'], 3, 768, scale_col=gq, skey='c_gq')
        self.load_w(wkv, 'c_wkv', io['w_ukv'], 2, 1024, scale_col=gkv, skey='c_gkv')
        qng_n, qng_r = fm[:, 0, 2:3], fm[0:64, 1, 2:3]
        kng_n, kng_r = fm[:, 0, 3:4], fm[0:64, 1, 3:4]
        knT = P.sb("c_knT", [128, NT], BF16)
        krh = P.sb("c_krh", [64, NT], BF16)
        vh = P.sb("c_vh", [128, NT // 128, 128], BF16)
        qnT = P.sb("c_qnT", [128, Q1], BF16)
        qrh = P.sb("c_qrh", [64, Q1], BF16)
        sqkv = Ring(P, "c_sqkv", [128, 2, 512], BF16, 2)
        sq3 = Ring(P, "c_sq3", [128, 3, 512], BF16, 2)
        sqb = Ring(P, "c_sqb", [128, 512], BF16, 3)
        f1 = Ring(P, "c_f", [128, 512], F32, 8)
        small = Ring(P, "c_small", [128, 2], F32, 4)
        pT = Ring(P, "c_pT", [128, 512], BF16, 3)
        SC = float(192 ** -0.5)
        for h in range(MH):
            P.barrier()
            for gi, (t0, w) in enumerate(self.gA):
                sk, skk = sqkv.next()
                for c in range(2):
                    P.act(sk[:, c, 0:w], self.kvcT[:, c, t0:t0 + w], AF.Square, w=[skk])
                for c in range(2):
                    P.mm(pb[0][:, 0:w], self.onesb[:], sk[:, c, 0:w], start=(c == 0), stop=(c == 1), r=['onesb', skk], w=['pb0'])
                rkv2, rkv2k = f1.next()
                P.ts(rkv2[:, 0:w], pb[0][:, 0:w], 1.0 / KVR, ALU.mult, EPS, ALU.add, r=['pb0'], w=[rkv2k])
                P.recip(rkv2[:, 0:w], rkv2[:, 0:w], r=[rkv2k], w=[rkv2k])
                rkv, rkvk = f1.next()
                P.act(rkv[:, 0:w], rkv2[:, 0:w], AF.Sqrt, r=[rkv2k], w=[rkvk])
                s2, s2k = sqb.next()
                P.act(s2[0:64, 0:w], self.krT[0:64, t0:t0 + w], AF.Square, w=[s2k])
                P.mm(pb[1][:, 0:w], self.onesb[0:64, :], s2[0:64, 0:w], r=['onesb', s2k], w=['pb1'])
                sskr, sskrk = f1.next()
                P.copy(sskr[:, 0:w], pb[1][:, 0:w], r=['pb1'], w=[sskrk], eng='act')
                for c in range(2):
                    P.mm(pb[2][:, 0:w], wkv[:, c, 256 * h:256 * h + 128], self.kvcT[:, c, t0:t0 + w],
                         start=(c == 0), stop=(c == 1), r=['c_wkv'], w=['pb2'])
                s3, s3k = sqb.next()
                P.act(s3[:, 0:w], pb[2][:, 0:w], AF.Square, r=['pb2'], w=[s3k])
                P.mm(pb[3][:, 0:w], self.onesb[:], s3[:, 0:w], r=['onesb', s3k], w=['pb3'])
                kt, ktk = f1.next()
                P.tt(kt[:, 0:w], pb[3][:, 0:w], rkv2[:, 0:w], ALU.mult, r=['pb3', rkv2k], w=[ktk])
                P.tt(kt[:, 0:w], kt[:, 0:w], sskr[:, 0:w], ALU.add, r=[ktk, sskrk], w=[ktk])
                P.rsqrt(kt[:, 0:w], kt[:, 0:w], 1.0 / 192, r=[ktk], w=[ktk])
                cb, cbk = f1.next()
                P.tt(cb[:, 0:w], kt[:, 0:w], rkv[:, 0:w], ALU.mult, r=[ktk, rkvk], w=[cbk])
                P.stt(knT[:, t0:t0 + w], pb[2][:, 0:w], kng_n, cb[:, 0:w], ALU.mult, ALU.mult,
                      r=['pb2', cbk, 'c_fm'], w=[('knT', gi)])
                if t0 < C:
                    P.stt(krh[0:64, t0:t0 + w], self.krT[0:64, t0:t0 + w], kng_r, kt[0:64, 0:w], ALU.mult, ALU.mult,
                          r=[ktk, 'c_fm'], w=[('krh', gi)])
                else:
                    kn_, knk = sqb.next()
                    P.stt(kn_[0:64, 0:w], self.krT[0:64, t0:t0 + w], kng_r, kt[0:64, 0:w], ALU.mult, ALU.mult,
                          r=[ktk, 'c_fm'], w=[knk])
                    self.rope_fm(krh[0:64, t0:t0 + w], kn_[0:64, 0:w], 64, w, t0 - C, f1, knk, ('krh', gi), 1)
                for ti in range(w // 128):
                    a0 = t0 + ti * 128
                    pv = pb[4 + (ti % 2)]
                    for c in range(2):
                        P.mm(pv[:, 0:128], self.kvcT[:, c, a0:a0 + 128], wkv[:, c, 256 * h + 128:256 * h + 256],
                             start=(c == 0), stop=(c == 1), r=['c_wkv'], w=[f'pb{4 + ti % 2}'])
                    for c in range(2):
                        P.mm(pb[6][:, 0:2], sk[:, c, ti * 128:(ti + 1) * 128], self.onesb[:, 0:2],
                             start=(c == 0), stop=(c == 1), r=['onesb', skk], w=['pb6'])
                    sm, smk = small.next()
                    P.rsqrt(sm[:, 0:1], pb[6][:, 0:1], 1.0 / KVR, r=['pb6'], w=[smk])
                    P.ts(vh[:, a0 // 128, :], pv[:, 0:128], sm[:, 0:1], ALU.mult, r=[f'pb{4 + ti % 2}', smk], w=[('vh', a0)])
            for gi, (t0, w) in enumerate(self.gQ):
                s3_, s3k_ = sq3.next()
                for c in range(3):
                    P.act(s3_[:, c, 0:w], self.qcT[:, c, t0:t0 + w], AF.Square, w=[s3k_])
                for c in range(3):
                    P.mm(pb[0][:, 0:w], self.onesb[:], s3_[:, c, 0:w], start=(c == 0), stop=(c == 2), r=['onesb', s3k_], w=['pb0'])
                eq, eqk = f1.next()
                P.ts(eq[:, 0:w], pb[0][:, 0:w], EPS / QR, ALU.mult, EPS * EPS, ALU.add, r=['pb0'], w=[eqk])
                for c in range(3):
                    P.mm(pb[2][:, 0:w], wq[:, c, 192 * h:192 * h + 128], self.qcT[:, c, t0:t0 + w],
                         start=(c == 0), stop=(c == 2), r=['c_wq'], w=['pb2'])
                for c in range(3):
                    P.mm(pb[3][0:64, 0:w], wq[:, c, 192 * h + 128:192 * h + 192], self.qcT[:, c, t0:t0 + w],
                         start=(c == 0), stop=(c == 2), r=['c_wq'], w=['pb3'])
                sa, sak = sqb.next()
                sb_, sbk = sqb.next()
                P.act(sa[:, 0:w], pb[2][:, 0:w], AF.Square, r=['pb2'], w=[sak])
                P.act(sb_[0:64, 0:w], pb[3][0:64, 0:w], AF.Square, r=['pb3'], w=[sbk])
                P.mm(pb[1][:, 0:w], self.onesb[:], sa[:, 0:w], start=True, stop=False, r=['onesb', sak], w=['pb1'])
                P.mm(pb[1][:, 0:w], self.onesb[0:64, :], sb_[0:64, 0:w], start=False, stop=True, r=['onesb', sbk], w=['pb1'])
                qs, qsk = f1.next()
                P.stt(qs[:, 0:w], pb[1][:, 0:w], 1.0 / 192, eq[:, 0:w], ALU.mult, ALU.add, r=['pb1', eqk], w=[qsk])
                P.act(qs[:, 0:w], qs[:, 0:w], AF.Sqrt, r=[qsk], w=[qsk])
                P.recip(qs[:, 0:w], qs[:, 0:w], r=[qsk], w=[qsk])
                P.stt(qnT[:, t0:t0 + w], pb[2][:, 0:w], qng_n, qs[:, 0:w], ALU.mult, ALU.mult, r=['pb2', qsk, 'c_fm'], w=[('qnT', gi)])
                if t0 < C:
                    P.stt(qrh[0:64, t0:t0 + w], pb[3][0:64, 0:w], qng_r, qs[0:64, 0:w], ALU.mult, ALU.mult,
                          r=['pb3', qsk, 'c_fm'], w=[('qrh', gi)])
                else:
                    qn_, qnk = sqb.next()
                    P.stt(qn_[0:64, 0:w], pb[3][0:64, 0:w], qng_r, qs[0:64, 0:w], ALU.mult, ALU.mult,
                          r=['pb3', qsk, 'c_fm'], w=[qnk])
                    self.rope_fm(qrh[0:64, t0:t0 + w], qn_[0:64, 0:w], 64, w, t0 - C, f1, qnk, ('qrh', gi), 0)
            P.barrier()
            for qi, (t0, w) in enumerate(self.gQ):
                nkt = (C // 128) if t0 < C else (NT // 128)
                po, pl = pb[4 + 2 * (qi % 2)], pb[5 + 2 * (qi % 2)]
                pok, plk = f'pb{4 + 2 * (qi % 2)}', f'pb{5 + 2 * (qi % 2)}'
                for kt_ in range(nkt):
                    k0 = kt_ * 128
                    bi = kt_ % 4
                    P.mm(pb[bi][:, 0:w], knT[:, k0:k0 + 128], qnT[:, t0:t0 + w], start=True, stop=False, w=[f'pb{bi}'])
                    P.mm(pb[bi][:, 0:w], krh[0:64, k0:k0 + 128], qrh[0:64, t0:t0 + w], start=False, stop=True, w=[f'pb{bi}'])
                    p_, pk = pT.next()
                    P.act(p_[:, 0:w], pb[bi][:, 0:w], AF.Exp, scale=SC, r=[f'pb{bi}'], w=[pk])
                    P.mm(po[:, 0:w], vh[:, kt_, :], p_[:, 0:w], start=(kt_ == 0), stop=(kt_ == nkt - 1), r=[pk], w=[pok])
                    P.mm(pl[:, 0:w], self.onesb[:], p_[:, 0:w], start=(kt_ == 0), stop=(kt_ == nkt - 1), r=[pk], w=[plk])
                rl, rlk = f1.next()
                P.recip(rl[:, 0:w], pl[:, 0:w], r=[plk], w=[rlk])
                ao, aok = aor.next()
                P.tt(ao[:, 0:w], po[:, 0:w], rl[:, 0:w], ALU.mult, r=[pok, rlk], w=[aok])
                P.dma(io['mix'][4 + h, :, t0:t0 + w], ao[:, 0:w], r=[aok])
        P.pop()
        P.pop()

    def phase_out(self, w_out_name):
        P, io, pb = self.P, self.io, self.pb
        Q0, Q1, TQ, NE, C = self.Q0, self.Q1, self.TQ, self.NE, self.C
        P.push()
        mixr = Ring(P, "d_mix", [128, 8, 128], BF16, 2)
        h2r = Ring(P, "d_h2", [128, 8, 128], BF16, 2)
        wout = P.sb("d_wout", [128, 8, D], BF16)
        wr = P.sb("d_wr", [128, 8, NE], F32)
        rb = P.sb("d_rb", [1, NE], F32)
        P.dma(wr[:], io['router_w'].rearrange("(c p) e -> p c e", p=128), w=['d_wr'])
        P.dma(rb[:], io['router_b'][:, :], w=['d_rb'])
        self.load_w(wout, 'd_wout', io[w_out_name], 8, D)
        xr = Ring(P, "d_xt", [128, D], F32, 2)
        x1r = Ring(P, "d_x1", [128, D], F32, 2)
        xnr = Ring(P, "d_xn", [128, D], F32, 2)
        jr = Ring(P, "d_junk", [128, D], BF16, 1)
        str_ = Ring(P, "d_stat", [128, 2], F32, 2)
        h32r = Ring(P, "d_h32", [128, 8, 128], F32, 2)
        lgr = Ring(P, "d_lg", [128, 4, NE], F32, 2)
        t8r = Ring(P, "d_t8", [128, 16], F32, 2)
        gtr = Ring(P, "d_gt", [NE, 128], F32, 2)
        for ti in range(TQ // 128):
            q0 = ti * 128
            a0 = Q0 + q0
            v = 1 if a0 < C else 0
            pa, pbk = (0, 1) if ti % 2 == 0 else (2, 3)
            mx, mxk = mixr.next()
            P.dma(mx[:], io['mix'][:, :, q0:q0 + 128].rearrange("c p t -> p c t"), w=[mxk])
            for hf, bi in enumerate((pa, pbk)):
                for c in range(8):
                    P.mm(pb[bi][:, :], mx[:, c, :], wout[:, c, hf * 512:(hf + 1) * 512],
                         start=(c == 0), stop=(c == 7), r=[mxk, 'd_wout'], w=[f'pb{bi}'])
            xt, xk = xr.next()
            P.dma(xt[:], self.xrow(a0), w=[xk])
            x1, x1k = x1r.next()
            for hf, bi in enumerate((pa, pbk)):
                P.tt(x1[:, hf * 512:(hf + 1) * 512], pb[bi][:, :], self.Gb[:, 0, v, hf * 512:(hf + 1) * 512], ALU.mult,
                     r=[f'pb{bi}', 'Gb'], w=[x1k])
            P.tt(x1[:], x1[:], xt[:], ALU.add, r=[x1k, xk], w=[x1k], eng='pool')
            P.dma(io['x1s'][q0:q0 + 128, :], x1[:], r=[x1k])
            jk, jkk = jr.next()
            st, stk = str_.next()
            P.act(jk[:], x1[:], AF.Square, accum=st[:, 0:1], r=[x1k], w=[jkk, stk])
            P.rsqrt(st[:, 1:2], st[:, 0:1], 1.0 / D, r=[stk], w=[stk])
            xn, xnk = xnr.next()
            P.ts(xn[:], x1[:], st[:, 1:2], ALU.mult, r=[x1k, stk], w=[xnk])
            for c in range(8):
                bi = 4 + c // 4
                P.tr(pb[bi][:, (c % 4) * 128:(c % 4 + 1) * 128], xn[:, c * 128:(c + 1) * 128], self.ident[:],
                     r=[xnk, 'ident'], w=[f'pb{bi}'])
            h32, hk = h32r.next()
            for c in range(8):
                bi = 4 + c // 4
                P.act(h32[:, c, :], pb[bi][:, (c % 4) * 128:(c % 4 + 1) * 128], AF.Identity,
                      scale=self.AB[:, 2, v, c:c + 1], bias=self.AB[:, 3, v, c:c + 1], r=[f'pb{bi}', 'AB'], w=[hk])
            h2, h2k = h2r.next()
            P.copy(h2[:], h32[:], r=[hk], w=[h2k], eng='pool')
            P.dma(io['h2s'][:, :, q0:q0 + 128].rearrange("c p t -> p c t"), h2[:], r=[h2k])
            for c in range(8):
                P.mm(pb[6][:, 0:NE], h32[:, c, :], wr[:, c, :], start=(c == 0), stop=False, r=[hk, 'd_wr'], w=['pb6'])
            P.mm(pb[6][:, 0:NE], self.ones32[0:1, 0:128], rb[0:1, :], start=False, stop=True, r=['ones32', 'd_rb'], w=['pb6'])
            lg, lgk = lgr.next()
            t8, t8k = t8r.next()
            P.copy(lg[:, 0, :], pb[6][:, 0:NE], r=['pb6'], w=[lgk])
            P.op('dve', lambda e, o=t8[:, 0:8], i=lg[:, 0, :]: e.max(out=o, in_=i), r=[lgk], w=[t8k])
            P.ts(lg[:, 1, :], lg[:, 0, :], t8[:, TOPK - 1:TOPK], ALU.is_ge, r=[lgk, t8k], w=[lgk])
            P.ts(t8[:, 8:9], t8[:, 0:1], -1.0, ALU.mult, r=[t8k], w=[t8k])
            P.act(lg[:, 2, :], lg[:, 0, :], AF.Exp, bias=t8[:, 8:9], r=[lgk, t8k], w=[lgk])
            P.tt(lg[:, 2, :], lg[:, 2, :], lg[:, 1, :], ALU.mult, r=[lgk], w=[lgk])
            P.op('dve', lambda e, o=t8[:, 9:10], i=lg[:, 2, :]: e.reduce_sum(out=o, in_=i, axis=AX.X), r=[lgk, t8k], w=[t8k])
            P.recip(t8[:, 10:11], t8[:, 9:10], r=[t8k], w=[t8k])
            P.ts(lg[:, 3, :], lg[:, 2, :], t8[:, 10:11], ALU.mult, r=[lgk, t8k], w=[lgk])
            P.tr(pb[7][0:NE, 0:128], lg[:, 3, :], self.ident[:], r=[lgk, 'ident'], w=['pb7'])
            gt, gtk = gtr.next()
            P.copy(gt[:], pb[7][0:NE, 0:128], r=['pb7'], w=[gtk], eng='act')
            P.dma(io['gates'][:, q0:q0 + 128], gt[:], r=[gtk])
        P.pop()

    def phase_moe(self, out_rows):
        P, io, pb = self.P, self.io, self.pb
        Q0, TQ, NE, C = self.Q0, self.TQ, self.NE, self.C
        P.push()
        ntile = TQ // 128
        npass = -(-ntile // 9) if ntile > 2 else ntile
        base, extra = ntile // npass, ntile % npass
        psizes = [(base + (1 if i < extra else 0)) * 128 for i in range(npass)]
        pstarts = [sum(psizes[:i]) for i in range(npass)]
        acc = P.sb("e_acc", [128, 8, max(psizes)], F32)
        hl2 = P.sb("e_hl2", [128, 8, max(psizes)], BF16)
        brow = P.sb("e_brow", [NE, 3 * FF], F32)
        bfm = P.sb("e_bfm", [128, 16, NE], F32)
        bd32 = brow[0:NE, 2 * FF:3 * FF]
        P.dma(brow[0:NE, 0:2 * FF], io['b_gu'][:, :], w=['e_brow'])
        P.dma(bd32, io['b_down'][:, :], w=['e_brow'])
        self.rows_to_fm(brow, NE, 2 * FF, bfm[:], 'e_brow', 'e_bfm')
        wgr = Ring(P, "e_wg", [128, 8, 1024], BF16, 2)
        wdr = Ring(P, "e_wd", [128, 4, D], BF16, 2)
        gbr = Ring(P, "e_gb", [128, 512], F32, 2)
        gtr = Ring(P, "e_gt", [NE, 512], F32, 2)
        e1 = Ring(P, "e_gcl", [128, 512], F32, 2)
        e2 = Ring(P, "e_sig", [128, 512], F32, 2)
        e3 = Ring(P, "e_ucl", [128, 512], F32, 2)
        agr = Ring(P, "e_actg", [128, 4, 512], BF16, 2)
        xr = Ring(P, "e_x1", [128, D], F32, 2)
        orr = Ring(P, "e_o", [128, D], F32, 2)
        kq = 0
        for tp in range(npass):
            p0, TP = pstarts[tp], psizes[tp]
            grp = groups(p0, p0 + TP)
            for c in range(8):
                P.dma(hl2[:, c, 0:TP], io['h2s'][c, :, p0:p0 + TP], w=['e_hl2'])
            for (t0, w) in grp:
                gt, gtk = gtr.next()
                P.dma(gt[:, 0:w], io['gates'][:, t0:t0 + w], w=[gtk])
                for dc in range(8):
                    bi = dc % 4
                    P.mm(pb[bi][:, 0:w], bd32[:, dc * 128:(dc + 1) * 128], gt[:, 0:w], r=['e_brow', gtk], w=[f'pb{bi}'])
                    P.copy(acc[:, dc, t0 - p0:t0 - p0 + w], pb[bi][:, 0:w], r=[f'pb{bi}'], w=[('acc', dc, t0)],
                           eng=('act' if dc % 2 else 'dve'))
            hes = [(e, hf) for e in range(NE) for hf in range(2)]

            def load_he(e, hf):
                wg, wgk = wgr.next()
                wd, wdk = wdr.next()
                for c in range(8):
                    for part in range(2):
                        st, sk = self.staged.next()
                        col0 = part * FF + hf * 512
                        P.dma(st[:, 0:512], io['w_gu'][e, c * 128:(c + 1) * 128, col0:col0 + 512], w=[sk])
                        P.copy(wg[:, c, part * 512:(part + 1) * 512], st[:, 0:512], r=[sk], w=[wgk], eng='pool')
                for c in range(4):
                    st, sk = self.staged.next()
                    r0 = hf * 512 + c * 128
                    P.dma(st[:, 0:D], io['w_down'][e, r0:r0 + 128, :], w=[sk])
                    P.copy(wd[:, c, :], st[:, 0:D], r=[sk], w=[wdk], eng='pool')
                return wg, wgk, wd, wdk

            def down(pend):
                ag, agk, t0, w, wd, wdk = pend
                for dc in range(8):
                    bi = 4 + dc % 4
                    for fc in range(4):
                        P.mm(pb[bi][:, 0:w], wd[:, fc, dc * 128:(dc + 1) * 128], ag[:, fc, 0:w],
                             start=(fc == 0), stop=(fc == 3), r=[wdk, agk], w=[f'pb{bi}'])
                    a_ap = acc[:, dc, t0 - p0:t0 - p0 + w]
                    P.tt(a_ap, pb[bi][:, 0:w], a_ap, ALU.add, r=[f'pb{bi}', ('acc', dc, t0)], w=[('acc', dc, t0)])

            nxt = load_he(*hes[0])
            pending = None
            for i, (e, hf) in enumerate(hes):
                wg, wgk, wd, wdk = nxt
                for gidx, (t0, w) in enumerate(grp):
                    gb, gbk = gbr.next()
                    P.dma(gb[:, 0:w], io['gates'][e:e + 1, t0:t0 + w].partition_broadcast(128), w=[gbk], q='act')
                    ag, agk = agr.next()
                    for fc in range(4):
                        kq += 1
                        bg, bu = (kq % 2) * 2, (kq % 2) * 2 + 1
                        for c in range(8):
                            P.mm(pb[bg][:, 0:w], wg[:, c, fc * 128:(fc + 1) * 128], hl2[:, c, t0 - p0:t0 - p0 + w],
                                 start=(c == 0), stop=(c == 7), r=[wgk, 'e_hl2'], w=[f'pb{bg}'])
                        for c in range(8):
                            P.mm(pb[bu][:, 0:w], wg[:, c, 512 + fc * 128:512 + (fc + 1) * 128], hl2[:, c, t0 - p0:t0 - p0 + w],
                                 start=(c == 0), stop=(c == 7), r=[wgk, 'e_hl2'], w=[f'pb{bu}'])
                        fg = hf * 4 + fc
                        gcl, gck = e1.next()
                        sg, sgk = e2.next()
                        ucl, uck = e3.next()
                        P.ts(gcl[:, 0:w], pb[bg][:, 0:w], bfm[:, fg, e:e + 1], ALU.add, LIMIT, ALU.min,
                             r=[f'pb{bg}', 'e_bfm'], w=[gck])
                        P.act(sg[:, 0:w], gcl[:, 0:w], AF.Sigmoid, scale=ALPHA, r=[gck], w=[sgk])
                        P.ts(ucl[:, 0:w], pb[bu][:, 0:w], bfm[:, 8 + fg, e:e + 1], ALU.add, LIMIT, ALU.min,
                             r=[f'pb{bu}', 'e_bfm'], w=[uck])
                        P.ts(ucl[:, 0:w], ucl[:, 0:w], -LIMIT, ALU.max, 1.0, ALU.add, r=[uck], w=[uck])
                        P.tt(ucl[:, 0:w], ucl[:, 0:w], gb[:, 0:w], ALU.mult, r=[uck, gbk], w=[uck])
                        P.tt(gcl[:, 0:w], gcl[:, 0:w], sg[:, 0:w], ALU.mult, r=[gck, sgk], w=[gck])
                        P.tt(ag[:, fc, 0:w], gcl[:, 0:w], ucl[:, 0:w], ALU.mult, r=[gck, uck], w=[agk])
                    if pending is not None:
                        down(pending)
                    pending = (ag, agk, t0, w, wd, wdk)
                    if gidx == 0 and i + 1 < len(hes):
                        nxt = load_he(*hes[i + 1])
            down(pending)
            for ti in range(TP // 128):
                q0 = p0 + ti * 128
                a0 = Q0 + q0
                v = 1 if a0 < C else 0
                for c in range(8):
                    bi = (ti % 2) * 2 + c // 4
                    keys = [('acc', c, t0) for (t0, w) in grp if t0 <= q0 < t0 + w]
                    P.tr(pb[bi][:, (c % 4) * 128:(c % 4 + 1) * 128], acc[:, c, ti * 128:(ti + 1) * 128], self.ident[:],
                         r=keys + ['ident'], w=[f'pb{bi}'])
                x1, x1k = xr.next()
                P.dma(x1[:], io['x1s'][q0:q0 + 128, :], w=[x1k])
                o, ok_ = orr.next()
                for hf in range(2):
                    bi = (ti % 2) * 2 + hf
                    P.tt(o[:, hf * 512:(hf + 1) * 512], pb[bi][:, :], self.Gb[:, 1, v, hf * 512:(hf + 1) * 512], ALU.mult,
                         r=[f'pb{bi}', 'Gb'], w=[ok_])
                P.tt(o[:], o[:], x1[:], ALU.add, r=[ok_, x1k], w=[ok_], eng='pool')
                for (dst, qs, n) in out_rows:
                    if qs <= q0 < qs + n:
                        P.dma(dst[q0 - qs:q0 - qs + 128, :], o[:], r=[ok_])
            P.barrier()
        P.pop()

    def phase_diff(self):
        import math
        P, io, pb = self.P, self.io, self.pb
        NT, C, OWN = self.NT, self.C, self.OWN
        lam_init = 0.8 - 0.6 * math.exp(-0.3 * self.layer)
        gw = min(512, OWN)
        qgroups = groups(0, OWN, gw)
        P.push()
        hT = P.sb("f_hT", [128, 8, NT], BF16)
        qTa = P.sb("f_qTa", [128, DH, OWN], BF16)
        self.ropr = Ring(P, "ropr", [128, 512], F32, 4)
        rows = P.sb("f_rows", [3, 128], F32)
        gfm = P.sb("f_gfm", [128, 1, 3], F32)
        lrow = P.sb("f_lrow", [1, 4, 64], F32)
        lsc = P.sb("f_lsc", [1, 8], F32)
        nlam = P.sb("f_nlam", [128, 2], F32)
        sg = P.sb("f_sg", [128, 1], F32)
        sqb = Ring(P, "f_sqb", [128, 512], BF16, 3)
        f1 = Ring(P, "f_f", [128, 512], F32, 8)
        for hf in range(2):
            P.dma(rows[0:1, hf * 64:(hf + 1) * 64], io['dqn_g'][:, :], w=['f_rows'])
            P.dma(rows[1:2, hf * 64:(hf + 1) * 64], io['dkn_g'][:, :], w=['f_rows'])
        P.dma(rows[2:3, :], io['subln_g'][:, :], w=['f_rows'])
        P.dma(lrow[0:1, :, :], io['lqk'].rearrange("(o a) k -> o a k", o=1), w=['f_lrow'])
        self.rows_to_fm(rows, 3, 128, gfm[:], 'f_rows', 'f_gfm')
        P.ts(sg[:], gfm[:, 0, 2:3], 1.0 - lam_init, ALU.mult, r=['f_gfm'], w=['f_sg'])
        for i in range(2):
            P.tt(lrow[0:1, 2 * i, :], lrow[0:1, 2 * i, :], lrow[0:1, 2 * i + 1, :], ALU.mult, r=['f_lrow'], w=['f_lrow'])
            P.op('dve', lambda e, o=lsc[0:1, i:i + 1], a=lrow[0:1, 2 * i, :]: e.reduce_sum(out=o, in_=a, axis=AX.X),
                 r=['f_lrow'], w=['f_lsc'])
        P.act(lsc[0:1, 0:2], lsc[0:1, 0:2], AF.Exp, r=['f_lsc'], w=['f_lsc'])
        for i in range(2):
            P.tt(lsc[0:1, 2 + i:3 + i], lsc[0:1, 1:2], lsc[0:1, 0:1], ALU.subtract, r=['f_lsc'], w=['f_lsc'])
        P.ts(lsc[0:1, 2:4], lsc[0:1, 2:4], -lam_init, ALU.add, r=['f_lsc'], w=['f_lsc'])
        P.mm(pb[0][:, 0:2], self.ones32[0:1, 0:128], lsc[0:1, 2:4], r=['ones32', 'f_lsc'], w=['pb0'])
        P.copy(nlam[:], pb[0][:, 0:2], r=['pb0'], w=['f_nlam'])
        qng, kng = gfm[:, 0, 0:1], gfm[:, 0, 1:2]

        def qk_side(w, lhs, rhs, rkeys, gain, dst, dkey, rope):
            for c in range(8):
                P.mm(pb[2][:, 0:w], lhs(c), rhs(c), start=(c == 0), stop=(c == 7), r=rkeys, w=['pb2'])
            s3, s3k = sqb.next()
            P.act(s3[:, 0:w], pb[2][:, 0:w], AF.Square, r=['pb2'], w=[s3k])
            P.mm(pb[3][:, 0:w], self.bd64[:], s3[:, 0:w], r=['bd64', s3k], w=['pb3'])
            rs, rsk = f1.next()
            P.rsqrt(rs[:, 0:w], pb[3][:, 0:w], 1.0 / DHD, r=['pb3'], w=[rsk])
            if rope is None:
                P.stt(dst, pb[2][:, 0:w], gain, rs[:, 0:w], ALU.mult, ALU.mult, r=['pb2', rsk, 'f_gfm'], w=[dkey])
            else:
                kn_, knk = sqb.next()
                P.stt(kn_[:, 0:w], pb[2][:, 0:w], gain, rs[:, 0:w], ALU.mult, ALU.mult, r=['pb2', rsk, 'f_gfm'], w=[knk])
                self.rope_fm(dst, kn_[:, 0:w], 128, w, rope[0], f1, knk, dkey, 1, tabs=rope[1])

        P.push()
        rings = self.norm_rings()
        for gi, (t0, w) in enumerate(self.gA):
            self.norm_tiles_to_hT(self.kvrow, t0, w, hT[:, :, t0:t0 + w], ('hT', gi), 0, rings)
        wq = P.sb("f_wq", [128, 8, 1024], BF16)
        self.load_w(wq, 'f_wq', io['w_in'][:, 0:1024], 8, 1024)
        hqr = Ring(P, "f_hq", [128, 8, 512], BF16, 2)
        for gi, (t0, w) in enumerate(qgroups):
            hq, hqk = hqr.next()
            self.norm_tiles_to_hT(lambda p: io['xl0'][p:p + 128, :], t0, w, hq, hqk, 0, rings, isctx=0)
            for h in range(DH):
                qk_side(w, lambda c: wq[:, c, h * 128:(h + 1) * 128], lambda c: hq[:, c, 0:w], ['f_wq', hqk], qng,
                        qTa[:, h, t0:t0 + w], ('qTa', h, gi), (t0, ('rcos', 'rsin')))
        P.pop()
        whr = Ring(P, "f_wh", [128, 8, 256], BF16, 2)
        kT = P.sb("f_kT", [128, NT], BF16)
        vh = P.sb("f_vh", [128, NT // 128, 128], BF16)
        pT = Ring(P, "f_pT", [128, 512], BF16, 4)
        aor = Ring(P, "f_ao", [128, 512], BF16, 2)
        SC = float(DHD ** -0.5)
        for h in range(DH):
            wh, whk = whr.next()
            for c in range(8):
                for part in range(2):
                    st, sk = self.staged.next()
                    col0 = (part + 1) * 1024 + h * 128
                    P.dma(st[:, 0:128], io['w_in'][c * 128:(c + 1) * 128, col0:col0 + 128], w=[sk])
                    P.copy(wh[:, c, part * 128:(part + 1) * 128], st[:, 0:128], r=[sk], w=[whk], eng='pool')
            P.barrier()
            for gi, (t0, w) in enumerate(self.gA):
                qk_side(w, lambda c: wh[:, c, 0:128], lambda c: hT[:, c, t0:t0 + w], [whk], kng,
                        kT[:, t0:t0 + w], ('kT', gi), None if t0 < C else (t0 - C, ('rcosk', 'rsink')))
                for ti in range(w // 128):
                    a0 = t0 + ti * 128
                    bi = 4 + (ti % 2)
                    for c in range(8):
                        P.mm(pb[bi][:, 0:128], hT[:, c, a0:a0 + 128], wh[:, c, 128:256], start=(c == 0), stop=(c == 7),
                             r=[whk], w=[f'pb{bi}'])
                    P.copy(vh[:, a0 // 128, :], pb[bi][:, 0:128], r=[f'pb{bi}'], w=[('vh', a0)], eng='act')
            P.barrier()
            nkt = NT // 128
            for qi, (q0, w) in enumerate(qgroups):
                for kt_ in range(nkt):
                    k0 = kt_ * 128
                    b1, b2 = kt_ % 2, 2 + kt_ % 2
                    P.mm(pb[b1][:, 0:w], kT[0:64, k0:k0 + 128], qTa[0:64, h, q0:q0 + w], w=[f'pb{b1}'])
                    P.mm(pb[b2][:, 0:w], kT[64:128, k0:k0 + 128], qTa[64:128, h, q0:q0 + w], w=[f'pb{b2}'])
                    p1, p1k = pT.next()
                    p2, p2k = pT.next()
                    P.act(p1[:, 0:w], pb[b1][:, 0:w], AF.Exp, scale=SC, r=[f'pb{b1}'], w=[p1k])
                    P.act(p2[:, 0:w], pb[b2][:, 0:w], AF.Exp, scale=SC, r=[f'pb{b2}'], w=[p2k])
                    st_, sp_ = (kt_ == 0), (kt_ == nkt - 1)
                    P.mm(pb[4][:, 0:w], vh[:, kt_, :], p1[:, 0:w], start=st_, stop=sp_, r=[p1k], w=['pb4'])
                    P.mm(pb[5][:, 0:w], self.onesb[:], p1[:, 0:w], start=st_, stop=sp_, r=[p1k], w=['pb5'])
                    P.mm(pb[6][:, 0:w], vh[:, kt_, :], p2[:, 0:w], start=st_, stop=sp_, r=[p2k], w=['pb6'])
                    P.mm(pb[7][:, 0:w], self.onesb[:], p2[:, 0:w], start=st_, stop=sp_, r=[p2k], w=['pb7'])
                r1, r1k = f1.next()
                r2, r2k = f1.next()
                P.recip(r1[:, 0:w], pb[5][:, 0:w], r=['pb5'], w=[r1k])
                P.recip(r2[:, 0:w], pb[7][:, 0:w], r=['pb7'], w=[r2k])
                P.tt(r1[:, 0:w], pb[4][:, 0:w], r1[:, 0:w], ALU.mult, r=['pb4', r1k], w=[r1k])
                P.tt(r2[:, 0:w], pb[6][:, 0:w], r2[:, 0:w], ALU.mult, r=['pb6', r2k], w=[r2k])
                dd, ddk = f1.next()
                P.stt(dd[:, 0:w], r2[:, 0:w], nlam[:, 0:1], r1[:, 0:w], ALU.mult, ALU.add, r=[r1k, r2k, 'f_nlam'], w=[ddk])
                s3, s3k = sqb.next()
                P.act(s3[:, 0:w], dd[:, 0:w], AF.Square, r=[ddk], w=[s3k])
                P.mm(pb[0][:, 0:w], self.onesb[:], s3[:, 0:w], r=['onesb', s3k], w=['pb0'])
                rs, rsk = f1.next()
                P.rsqrt(rs[:, 0:w], pb[0][:, 0:w], 1.0 / 128, r=['pb0'], w=[rsk])
                ao, aok = aor.next()
                P.stt(ao[:, 0:w], dd[:, 0:w], sg[:, 0:1], rs[:, 0:w], ALU.mult, ALU.mult, r=[ddk, rsk, 'f_sg'], w=[aok])
                P.dma(io['mix'][h, :, q0:q0 + w], ao[:, 0:w], r=[aok])
            P.barrier()
        P.pop()


def build_fused(cfg, ncores):
    sh = LayerBuilder(cfg, 0)
    S, C, NE, NT, OWN = sh.S, sh.C, sh.NE, sh.NT, sh.OWN
    P = sh.P
    for nm, shp in [('ident', [128, 128]), ('prot', [128, 128]), ('rcos', [128, S]), ('rsin', [128, S]),
                    ('cc', [2, D]), ('xa', [NT, D])]:
        sh.din(nm, shp, glob=True)
    sh.gio['rcosk'], sh.gio['rsink'] = sh.io['rcos'], sh.io['rsin']
    sh.io['rcosk'], sh.io['rsink'] = sh.io['rcos'], sh.io['rsin']
    sh.dscr('xl0', [S, D], glob=True)
    sh.dscr('xc0', [C, D], glob=True)
    sh.dscr('mix', [8, 128, NT], BF16, glob=True)
    sh.dscr('h2s', [8, 128, NT], BF16, glob=True)
    sh.dscr('qkvs', [6, 128, NT], BF16, glob=True)
    sh.dscr('x1s', [NT, D], glob=True)
    sh.dscr('gates', [NE, NT], glob=True)
    out_l = sh.dout('out_l', [OWN, D])
    common = [('mod_w', [D, 6 * D]), ('mod_b', [1, 6 * D]), ('n1g', [1, D]), ('n2g', [1, D]),
              ('router_w', [D, NE]), ('router_b', [1, NE]), ('w_gu', [NE, D, 2 * FF]), ('b_gu', [NE, 2 * FF]),
              ('w_down', [NE, FF, D]), ('b_down', [NE, D]), ('w_out', [D, D])]
    L0 = sh
    for nm, shp in common + [('w_in', [D, EVEN_IN]), ('conv5', [5, 512]), ('conv_b', [1, 512]), ('lru_wa', [2, 8, 64, 64]),
                             ('lru_wx', [2, 8, 64, 64]), ('lru_ba', [2, 512]), ('lru_bx', [2, 512]), ('lru_lam', [2, 512]),
                             ('q_norm_g', [1, QR]), ('w_uq', [QR, 768]), ('kv_norm_g', [1, KVR]), ('w_ukv', [KVR, 1024]),
                             ('qn_g', [1, 192]), ('kn_g', [1, 192])]:
        L0.din(nm, shp)
    L0.setup_consts()
    P.push()
    L0.xrow = lambda a0: L0.io['xa'][a0:a0 + 128, :]
    L0.phase_mod()
    L0.phase_in_even()
    L0.phase_lru()
    L0.phase_mla()
    L0.phase_out('w_out')
    L0.phase_moe([(L0.io['xc0'], 0, C), (L0.io['xl0'], C, S)])
    P.pop()
    L1 = LayerBuilder(cfg, 1, shared=sh)
    for nm, shp in common + [('w_in', [D, 3072]), ('dqn_g', [1, 64]), ('dkn_g', [1, 64]), ('lqk', [4, 64]), ('subln_g', [1, 128])]:
        L1.din(nm, shp)
    P.push()
    L1.xrow = lambda a0: L1.io['xl0'][a0 - C:a0 - C + 128, :]
    L1.kvrow = lambda a0: (L1.io['xc0'][a0:a0 + 128, :] if a0 < C else L1.io['xl0'][a0 - C:a0 - C + 128, :])
    L1.phase_mod()
    L1.phase_diff()
    L1.phase_out('w_out')
    L1.phase_moe([(out_l, 0, OWN)])
    P.pop()
    P.finish()
    return sh


def rope_tables_np(S, GW=64, rot_dim=64, base=10000.0):
    n_rows = S // GW
    rows = np.repeat(np.arange(n_rows, dtype=np.float32), GW)
    cols = np.tile(np.arange(GW, dtype=np.float32), n_rows)
    axis_dim = rot_dim // 2
    inv_freq = (base ** (-np.arange(0, axis_dim, 2, dtype=np.float32) / axis_dim)).astype(np.float32)
    ang_r = rows[:, None] * inv_freq
    ang_c = cols[:, None] * inv_freq
    ang = np.concatenate([ang_r, ang_r, ang_c, ang_c], axis=-1)
    return np.cos(ang).astype(np.float32), np.sin(ang).astype(np.float32)


def prot_matrix():
    Pm = np.zeros((128, 128), np.float32)
    for blk in range(2):
        o = blk * 64
        for a in range(2):
            for q in range(16):
                Pm[o + a * 32 + 16 + q, o + a * 32 + q] = -1.0
                Pm[o + a * 32 + q, o + a * 32 + 16 + q] = 1.0
    return Pm


def core_inputs(inp, b, half, cfg, core=None, ncores=8):
    S, C = cfg['S'], cfg['C']
    core = 2 * b + half if core is None else core
    EPC = cfg['NE'] // ncores
    OWN = S // 2
    rev = (half == 1)
    f = (lambda a: a[::-1]) if rev else (lambda a: a)
    A = np.ascontiguousarray
    cos, sin = rope_tables_np(S)
    st2 = lambda t: A(np.concatenate([t.T, t.T], 0))
    pair_order = np.concatenate([np.arange(OWN), np.arange(S - 1, S - 1 - OWN, -1)])
    m = {
        'xa': A(np.concatenate([f(inp['ctx'][b]), f(inp['x'][b])], 0)),
        'cc': A(np.stack([inp['c'][b], inp['c_ctx']], 0)),
        'ident': np.eye(128, dtype=np.float32), 'prot': prot_matrix(),
        'rcos': st2(f(cos)), 'rsin': st2(f(sin)),
    }
    for layer in range(cfg['DEPTH']):
        i = layer // 2
        p = f"L{layer}_"
        m.update({
            p + 'mod_w': A(inp['mod_w'][layer]), p + 'mod_b': A(inp['mod_b'][layer][None]),
            p + 'n1g': A(inp['norm1_g'][layer][None]), p + 'n2g': A(inp['norm2_g'][layer][None]),
            p + 'router_w': A(inp['router_w'][layer]), p + 'router_b': A(inp['router_b'][layer][None]),
            p + 'w_gu': A(inp['moe_w_gu'][layer]), p + 'b_gu': A(inp['moe_b_gu'][layer]),
            p + 'w_down': A(inp['moe_w_down'][layer]), p + 'b_down': A(inp['moe_b_down'][layer]),
        })
        if layer % 2 == 0:
            cw = inp['lru_conv_w'][i]
            z = np.zeros((1, 512), np.float32)
            conv5 = np.concatenate([cw, z], 0) if not rev else np.concatenate([z, cw[::-1]], 0)
            m.update({
                p + 'w_in': A(inp['ev_w_in'][i]), p + 'w_out': A(inp['ev_w_out'][i]), p + 'conv5': A(conv5),
                p + 'conv_b': A(inp['lru_conv_b'][i][None]), p + 'lru_wa': A(f(inp['lru_wa'][i])),
                p + 'lru_wx': A(f(inp['lru_wx'][i])), p + 'lru_ba': A(f(inp['lru_ba'][i])),
                p + 'lru_bx': A(f(inp['lru_bx'][i])), p + 'lru_lam': A(f(inp['lru_lambda'][i])),
                p + 'q_norm_g': A(inp['mla_q_norm_g'][i][None]), p + 'w_uq': A(inp['mla_w_uq'][i]),
                p + 'kv_norm_g': A(inp['mla_kv_norm_g'][i][None]), p + 'w_ukv': A(inp['mla_w_ukv'][i]),
                p + 'qn_g': A(inp['mla_qn_g'][i][None]), p + 'kn_g': A(inp['mla_kn_g'][i][None]),
            })
        else:
            m.update({
                p + 'w_in': A(inp['od_w_in'][i]), p + 'w_out': A(inp['od_w_out'][i]),
                p + 'dqn_g': A(inp['diff_qn_g'][i][None]), p + 'dkn_g': A(inp['diff_kn_g'][i][None]),
                p + 'lqk': A(np.stack([inp['diff_lq1'][i], inp['diff_lk1'][i], inp['diff_lq2'][i], inp['diff_lk2'][i]], 0)),
                p + 'subln_g': A(inp['diff_subln_g'][i][None]),
            })
    return m


def gather_outputs(results, cfg, nb):
    S = cfg['S']
    OWN = S // 2
    xl = np.zeros((nb, S, D), np.float32)
    for core, r in enumerate(results):
        b, half = core // 2, core % 2
        if half == 0:
            xl[b, 0:OWN] = r['out_l']
        else:
            xl[b, S - OWN:S] = r['out_l'][::-1]
    return xl


CFG = dict(S=4096, C=256, NE=32, DEPTH=2)
_PROG = {}


def kernel(**inputs):
    inp = {k: np.asarray(v) for k, v in inputs.items()}
    nb = inp['x'].shape[0]
    ncore = 2 * nb
    if ncore not in _PROG:
        _PROG[ncore] = build_fused(CFG, ncore)
    B = _PROG[ncore]
    in_maps = [core_inputs(inp, c // 2, c % 2, CFG, core=c, ncores=ncore) for c in range(ncore)]
    res = run_bass_kernel_spmd(B.nc, in_maps, core_ids=list(range(ncore)))
    return gather_outputs(res.results, CFG, nb)
```
